# Optimizing a Trainium2 kernel written in Bass

```python
import math
import jax
import jax.numpy as jnp
from jax import lax
import numpy as np

D_MODEL = 1024
BATCH = 8
SEQ = 2048
DEPTH = 1

N_META = 16
EPS = 1e-6
DA_HEADS = 4
DA_HEAD_DIM = 64
DA_V_DIM = 2 * DA_HEAD_DIM
DA_WIDTH = DA_HEADS * DA_V_DIM
ROPE_THETA = 500000.0
ROPE_DIM = DA_HEAD_DIM // 4
Q_BLOCK = 128
GLA_HEADS = 4
GLA_DK = 64
GLA_DV = 128
GLA_WIDTH = GLA_HEADS * GLA_DV
GLA_GATE_RANK = 16
GLA_TAU = 16.0
GLA_CHUNK = 64
MIX_WIDTH = DA_WIDTH + GLA_WIDTH
IN_SIZES = (DA_HEADS * 2 * DA_HEAD_DIM, DA_HEADS * 2 * DA_HEAD_DIM, DA_WIDTH, GLA_HEADS * GLA_DK, GLA_HEADS * GLA_DK, GLA_WIDTH, GLA_WIDTH, GLA_GATE_RANK, GLA_GATE_RANK)
IN_WIDTH = sum(IN_SIZES)
PEER_HEADS = 8
PEER_NKEYS = 128
PEER_EXPERTS = PEER_NKEYS * PEER_NKEYS
PEER_KEY_DIM = 128
PEER_TOPK = 16
PEER_BLOCK = 256

kernel_name = 'hymba_diffattn_gla_peer_encoder'


def rmsnorm(x, w):
    xf = x.astype(jnp.float32)
    y = xf * lax.rsqrt(jnp.mean(xf * xf, axis=-1, keepdims=True) + EPS)
    return (y * w.astype(jnp.float32)).astype(x.dtype)


def partial_rope(x, pos):
    inv = jnp.power(ROPE_THETA, -jnp.arange(0, ROPE_DIM, 2, dtype=jnp.float32) / ROPE_DIM)
    ang = pos.astype(jnp.float32)[:, None] * inv[None, :]
    cos = jnp.cos(ang)[None, :, None, None, :]
    sin = jnp.sin(ang)[None, :, None, None, :]
    xr = x[..., :ROPE_DIM].astype(jnp.float32)
    x1, x2 = xr[..., :ROPE_DIM // 2], xr[..., ROPE_DIM // 2:]
    rot = jnp.concatenate([x1 * cos - x2 * sin, x2 * cos + x1 * sin], axis=-1)
    return jnp.concatenate([rot.astype(x.dtype), x[..., ROPE_DIM:]], axis=-1)


def diff_attention(q, k, v, lam, pos):
    B, T = q.shape[0], q.shape[1]
    q = partial_rope(q, pos) * (DA_HEAD_DIM ** -0.5)
    k = partial_rope(k, pos)
    nb = -(-T // Q_BLOCK)
    qp = jnp.pad(q, ((0, 0), (0, nb * Q_BLOCK - T), (0, 0), (0, 0), (0, 0)))
    qb = qp.reshape(B, nb, Q_BLOCK, DA_HEADS, 2, DA_HEAD_DIM).transpose(1, 0, 2, 3, 4, 5)

    def block(qi):
        s = jnp.einsum('bqhcd,bkhcd->bhcqk', qi, k).astype(jnp.float32)
        p = jax.nn.softmax(s, axis=-1)
        a = p[:, :, 0] - lam * p[:, :, 1]
        return jnp.einsum('bhqk,bkhv->bqhv', a.astype(v.dtype), v)

    o = lax.map(block, qb)
    return o.transpose(1, 0, 2, 3, 4).reshape(B, nb * Q_BLOCK, DA_HEADS, DA_V_DIM)[:, :T]


def gla_chunk_scan(q, k, v, g):
    B, L = q.shape[0], q.shape[1]
    n = L // GLA_CHUNK

    def to_chunks(a):
        return a.reshape(B, n, GLA_CHUNK, a.shape[2], a.shape[3]).transpose(1, 0, 3, 2, 4)

    mask = jnp.tril(jnp.ones((GLA_CHUNK, GLA_CHUNK), dtype=bool))[:, :, None]

    def step(state, inp):
        qc, kc, vc, gc = inp
        b = jnp.cumsum(gc, axis=2)
        rel = b[:, :, :, None, :] - b[:, :, None, :, :]
        decay = jnp.exp(jnp.where(mask, rel, -jnp.inf))
        attn = jnp.einsum('bhid,bhjd,bhijd->bhij', qc, kc, decay)
        o = jnp.einsum('bhij,bhjv->bhiv', attn, vc) + jnp.einsum('bhid,bhdv->bhiv', qc * jnp.exp(b), state)
        b_last = b[:, :, -1]
        state = state * jnp.exp(b_last)[..., None] + jnp.einsum('bhjd,bhjv->bhdv', kc * jnp.exp(b_last[:, :, None] - b), vc)
        return state, o

    state0 = jnp.zeros((B, q.shape[2], q.shape[3], v.shape[3]), jnp.float32)
    _, o = lax.scan(step, state0, (to_chunks(q), to_chunks(k), to_chunks(v), to_chunks(g)))
    return o.transpose(1, 0, 3, 2, 4).reshape(B, L, q.shape[2], v.shape[3])


def gla_bidirectional(q, k, v, g_f, g_b):
    pad = (-N_META) % GLA_CHUNK
    padf = lambda a: jnp.pad(a, ((0, 0), (pad, 0), (0, 0), (0, 0)))
    flip = lambda a: a[:, ::-1]
    qp, kp, vp = padf(q), padf(k), padf(v)
    o_f = gla_chunk_scan(qp, kp, vp, padf(g_f))
    o_b = flip(gla_chunk_scan(flip(qp), flip(kp), flip(vp), flip(padf(g_b))))
    return (o_f + o_b)[:, pad:]


def peer_ffn(x, w_q, sub_keys, u, v):
    N, D = x.shape
    nb = -(-N // PEER_BLOCK)
    xb = jnp.pad(x, ((0, nb * PEER_BLOCK - N), (0, 0))).reshape(nb, PEER_BLOCK, D)

    def block(xi):
        n = xi.shape[0]
        q = (xi @ w_q).reshape(n, PEER_HEADS, 2, PEER_KEY_DIM)
        s = jnp.einsum('nhpc,hpkc->nhpk', q, sub_keys).astype(jnp.float32)
        ts, ti = lax.top_k(s, PEER_TOPK)
        cand_s = (ts[:, :, 0, :, None] + ts[:, :, 1, None, :]).reshape(n, PEER_HEADS, PEER_TOPK * PEER_TOPK)
        cand_i = (ti[:, :, 0, :, None] * PEER_NKEYS + ti[:, :, 1, None, :]).reshape(n, PEER_HEADS, PEER_TOPK * PEER_TOPK)
        best_s, best_p = lax.top_k(cand_s, PEER_TOPK)
        idx = jnp.take_along_axis(cand_i, best_p, axis=-1)
        gate = jax.nn.softmax(best_s, axis=-1)
        act = jax.nn.gelu(jnp.einsum('nd,nhkd->nhk', xi, u[idx]).astype(jnp.float32), approximate=False)
        return jnp.einsum('nhk,nhkd->nd', (gate * act).astype(xi.dtype), v[idx])

    return lax.map(block, xb).reshape(nb * PEER_BLOCK, D)[:N]


def setup_inputs(seed: int = 0) -> dict:
    key = jax.random.key(seed)
    ks = jax.random.split(key, 24)
    nrm = lambda k, shape, s: jax.random.normal(k, shape, jnp.float32) * s
    gain = lambda k, shape: 1.0 + 0.02 * jax.random.normal(k, shape, jnp.float32)
    return {
        'x': nrm(ks[0], (BATCH, SEQ, D_MODEL), 1.0),
        'meta_tokens': nrm(ks[1], (N_META, D_MODEL), 1.0),
        'ln1_w': gain(ks[2], (DEPTH, D_MODEL)),
        'w_in': nrm(ks[3], (DEPTH, D_MODEL, IN_WIDTH), D_MODEL ** -0.5),
        'da_lambda_q1': nrm(ks[4], (DEPTH, DA_HEAD_DIM), 0.1),
        'da_lambda_k1': nrm(ks[5], (DEPTH, DA_HEAD_DIM), 0.1),
        'da_lambda_q2': nrm(ks[6], (DEPTH, DA_HEAD_DIM), 0.1),
        'da_lambda_k2': nrm(ks[7], (DEPTH, DA_HEAD_DIM), 0.1),
        'da_subln_w': gain(ks[8], (DEPTH, DA_V_DIM)),
        'gla_gate_w2_f': nrm(ks[9], (DEPTH, GLA_GATE_RANK, GLA_HEADS * GLA_DK), GLA_GATE_RANK ** -0.5),
        'gla_gate_b_f': nrm(ks[10], (DEPTH, GLA_HEADS * GLA_DK), 0.1),
        'gla_gate_w2_b': nrm(ks[11], (DEPTH, GLA_GATE_RANK, GLA_HEADS * GLA_DK), GLA_GATE_RANK ** -0.5),
        'gla_gate_b_b': nrm(ks[12], (DEPTH, GLA_HEADS * GLA_DK), 0.1),
        'gla_norm_w': gain(ks[13], (DEPTH, GLA_DV)),
        'w_out': nrm(ks[14], (DEPTH, MIX_WIDTH, D_MODEL), MIX_WIDTH ** -0.5),
        'ln2_w': gain(ks[15], (DEPTH, D_MODEL)),
        'peer_w_q': nrm(ks[16], (DEPTH, D_MODEL, PEER_HEADS * 2 * PEER_KEY_DIM), D_MODEL ** -0.5),
        'peer_sub_keys': nrm(ks[17], (DEPTH, PEER_HEADS, 2, PEER_NKEYS, PEER_KEY_DIM), PEER_KEY_DIM ** -0.5),
        'peer_u': nrm(ks[18], (DEPTH, PEER_EXPERTS, D_MODEL), D_MODEL ** -0.5),
        'peer_v': nrm(ks[19], (DEPTH, PEER_EXPERTS, D_MODEL), PEER_HEADS ** -0.5),
        'final_norm_w': gain(ks[20], (D_MODEL,)),
    }


def reference(x, meta_tokens, ln1_w, w_in, da_lambda_q1, da_lambda_k1, da_lambda_q2, da_lambda_k2, da_subln_w, gla_gate_w2_f, gla_gate_b_f, gla_gate_w2_b, gla_gate_b_b, gla_norm_w, w_out, ln2_w, peer_w_q, peer_sub_keys, peer_u, peer_v, final_norm_w):
    B = x.shape[0]
    meta = jnp.broadcast_to(meta_tokens.astype(x.dtype)[None], (B, N_META, D_MODEL))
    h = jnp.concatenate([meta, x], axis=1)
    T = h.shape[1]
    pos = jnp.arange(T)
    splits = np.cumsum(IN_SIZES)[:-1].tolist()
    for l in range(DEPTH):
        xn = rmsnorm(h, ln1_w[l])
        proj = xn @ w_in[l]
        dq, dk, dv, gq, gk, gv, gr, zf, zb = jnp.split(proj, splits, axis=-1)

        lam_init = 0.8 - 0.6 * math.exp(-0.3 * l)
        lam = (jnp.exp(jnp.sum(da_lambda_q1[l].astype(jnp.float32) * da_lambda_k1[l].astype(jnp.float32)))
               - jnp.exp(jnp.sum(da_lambda_q2[l].astype(jnp.float32) * da_lambda_k2[l].astype(jnp.float32))) + lam_init)
        da = diff_attention(dq.reshape(B, T, DA_HEADS, 2, DA_HEAD_DIM), dk.reshape(B, T, DA_HEADS, 2, DA_HEAD_DIM),
                            dv.reshape(B, T, DA_HEADS, DA_V_DIM), lam, pos)
        da = (rmsnorm(da, da_subln_w[l]) * (1.0 - lam_init)).reshape(B, T, DA_WIDTH)

        f32 = jnp.float32
        log_g_f = jax.nn.log_sigmoid((zf @ gla_gate_w2_f[l] + gla_gate_b_f[l]).astype(f32)) / GLA_TAU
        log_g_b = jax.nn.log_sigmoid((zb @ gla_gate_w2_b[l] + gla_gate_b_b[l]).astype(f32)) / GLA_TAU
        go = gla_bidirectional((gq.astype(f32) * (GLA_DK ** -0.5)).reshape(B, T, GLA_HEADS, GLA_DK),
                               gk.astype(f32).reshape(B, T, GLA_HEADS, GLA_DK),
                               gv.astype(f32).reshape(B, T, GLA_HEADS, GLA_DV),
                               log_g_f.reshape(B, T, GLA_HEADS, GLA_DK),
                               log_g_b.reshape(B, T, GLA_HEADS, GLA_DK))
        go = rmsnorm(go, gla_norm_w[l]).reshape(B, T, GLA_WIDTH).astype(h.dtype) * jax.nn.silu(gr)

        h = h + jnp.concatenate([da, go], axis=-1) @ w_out[l]
        if l == DEPTH - 1:
            h = h[:, N_META:]

        hn = rmsnorm(h, ln2_w[l])
        h = h + peer_ffn(hn.reshape(-1, D_MODEL), peer_w_q[l], peer_sub_keys[l], peer_u[l], peer_v[l]).reshape(h.shape)
    return rmsnorm(h, final_norm_w)
```

```python
import numpy as np
import concourse.bass as bass
import concourse.mybir as mybir
from concourse.bass_utils import run_bass_kernel_spmd

F32 = mybir.dt.float32
BF16 = mybir.dt.bfloat16
I32 = mybir.dt.int32
U32 = mybir.dt.uint32
ALU = mybir.AluOpType
AF = mybir.ActivationFunctionType
AX = mybir.AxisListType

ENGS = ("pe", "act", "dve", "pool", "sp")
NSLOT = 12


class Prog:
    def __init__(self, nc, es):
        self.nc = nc
        self.q = {e: [] for e in ENGS}
        self.cnt = {e: 0 for e in ENGS}
        self.waited = {e: {} for e in ENGS}
        self.lastw = {}
        self.readers = {}
        self.pending = {e: ([], []) for e in ENGS}
        self.sems = {}
        for e in ENGS:
            self.sems[e] = es.enter_context(nc.semaphore("s_" + e))
        self.dslot = {}
        self.duse = {}
        self.dnext = {}
        for e in ("sp", "act", "pool"):
            self.dnext[e] = 0
            for i in range(NSLOT):
                k = ("d", e, i)
                self.sems[k] = es.enter_context(nc.semaphore("d_%s_%d" % (e, i)))
                self.duse[k] = 0
        self.final = []
        self.rec = None

    def _need(self, eng, dep, waits):
        if dep is None:
            return
        k, v = dep
        if k == "pe" and eng == "pe":
            return
        if self.waited[eng].get(k, 0) >= v:
            return
        self.waited[eng][k] = v
        waits.append((k, v))

    def _deps(self, eng, reads, writes):
        waits = []
        for r in reads:
            self._need(eng, self.lastw.get(r), waits)
        for w in writes:
            self._need(eng, self.lastw.get(w), waits)
            for d in self.readers.get(w, ()):
                self._need(eng, d, waits)
        return waits

    def _commit(self, token, reads, writes):
        for r in reads:
            self.readers.setdefault(r, []).append(token)
        for w in writes:
            self.lastw[w] = token
            self.readers[w] = []

    def op(self, eng, fn, reads=(), writes=(), inc=True):
        if self.rec is not None:
            self.rec.append(("op", eng, fn, list(reads), list(writes), inc))
            return
        waits = self._deps(eng, reads, writes)
        pr, pw = self.pending[eng]
        pr.extend(reads)
        pw.extend(writes)
        if inc:
            self.cnt[eng] += 1
            token = (eng, self.cnt[eng])
            self._commit(token, pr, pw)
            self.pending[eng] = ([], [])
            self.q[eng].append((waits, fn, eng, 1))
        else:
            self.q[eng].append((waits, fn, None, 0))

    def dma(self, eng, fn, reads=(), writes=(), final=False):
        if self.rec is not None:
            self.rec.append(("dma", eng, fn, list(reads), list(writes), final))
            return
        waits = self._deps(eng, reads, writes)
        i = self.dnext[eng]
        self.dnext[eng] = (i + 1) % NSLOT
        k = ("d", eng, i)
        if self.duse[k] > 0:
            self._need(eng, (k, 16 * self.duse[k]), waits)
        self.duse[k] += 1
        token = (k, 16 * self.duse[k])
        self._commit(token, reads, writes)
        self.q[eng].append((waits, fn, k, 16))
        if final:
            self.final.append(token)

    def replay(self, r):
        if r[0] == "op":
            self.op(r[1], r[2], r[3], r[4], r[5])
        else:
            self.dma(r[1], r[2], r[3], r[4], r[5])

    def barrier(self):
        toks = [(e, self.cnt[e]) for e in ENGS if self.cnt[e] > 0]
        for k, n in self.duse.items():
            if n > 0:
                toks.append((k, 16 * n))
        for e in ENGS:
            waits = []
            for tok in toks:
                self._need(e, tok, waits)
            if waits:
                self.q[e].append((waits, None, None, 0))

    def emit(self):
        nc = self.nc
        fw = []
        for tok in self.final:
            self._need("sp", tok, fw)
        self.q["sp"].append((fw, None, None, 0))
        with nc.Block() as block:
            def run(engname):
                def body(e):
                    for waits, fn, sk, iv in self.q[engname]:
                        for k, v in waits:
                            e.wait_ge(self.sems[k], v)
                        if fn is None:
                            continue
                        if isinstance(fn, tuple):
                            ins = getattr(e, fn[0])(*fn[1], **fn[2])
                        else:
                            ins = fn(e)
                        if sk is not None:
                            ins.then_inc(self.sems[sk], iv)
                return body

            block.sync(run("sp"))
            block.scalar(run("act"))
            block.vector(run("dve"))
            block.gpsimd(run("pool"))
            block.tensor(run("pe"))
from contextlib import ExitStack
import ml_dtypes

D = 1024
T_REAL = 2048
NMETA = 16
EPS = 1e-6
IN_W = 3104
NEG = -1.0e30


def I(name, *a, **k):
    return (name, a, k)


def rgroup(P, lst, i):
    P.replay(lst[i])
    i += 1
    while i < len(lst) and lst[i - 1][0] == "op" and lst[i - 1][1] == "pe" and lst[i - 1][5] is False:
        P.replay(lst[i])
        i += 1
    return i


def build(dbg=None):
    nc = bass.Bass("TRN2", target_bir_lowering=False)

    def din(name, shape, dt=F32):
        return nc.dram_tensor(name, list(shape), dt, kind="ExternalInput").ap()

    x = din("x", [2048, D])
    meta = din("meta", [16, D])
    w_in = din("w_in", [D, IN_W])
    ln1c = din("ln1c", [128, 8])
    w_out = din("w_out", [D, D])
    ln2c = din("ln2c", [128, 8])
    ln2_b = din("ln2_b", [128, D])
    fin_b = din("fin_b", [128, D])
    w_q = din("w_q", [D, 2048])
    keysT = din("keysT", [128, 2048])
    peer_uv = din("peer_uv", [16384, 2 * D])
    uv_bf = nc.dram_tensor("uv_bf", [16384, 2 * D], BF16, kind="Internal").ap()
    wq_dram = nc.dram_tensor("wq_dram", [D, 2048], BF16, kind="Internal").ap()
    wo_dram = nc.dram_tensor("wo_dram", [D, D], BF16, kind="Internal").ap()
    subln_b = din("subln_b", [128, 128])
    glan_b = din("glan_b", [128, 128])
    lamv = din("lamv", [128, 256])
    w2cat = din("w2cat", [32, 512])
    bcat = din("bcat", [1, 512])
    c_identb = din("c_identb", [128, 128], BF16)
    c_identf = din("c_identf", [128, 128])
    c_tri = din("c_tri", [64, 256])
    c_cs = din("c_cs", [128, 17 * 16])
    c_iota = din("c_iota", [128, 256])
    out = nc.dram_tensor("out", [2048, D], F32, kind="ExternalOutput").ap()
    dbg_out = None
    if dbg is not None:
        dbg_out = nc.dram_tensor("dbg", list(dbg[1]), F32, kind="ExternalOutput").ap()

    es = ExitStack()
    with es:
        P = Prog(nc, es)

        def sb(name, shape, dt=F32, stack=es):
            return stack.enter_context(nc.sbuf_tensor(name, list(shape), dt))

        psb = [es.enter_context(nc.psum_tensor("ps%d" % i, [128, 512], F32)) for i in range(8)]

        def PSK(i):
            return "ps%d" % i

        identb = sb("identb", [128, 128], BF16)
        identf = sb("identf", [128, 128])
        iota = sb("iota", [128, 256])
        ln2 = sb("ln2", [128, 8])
        mix_da = sb("mix_da", [128, 16, 512], BF16)
        og_full = sb("og", [128, 32, 512], BF16)
        og = og_full[0:64]
        stat = sb("stat", [128, 16])
        junkb = sb("junkb", [128, 1024], BF16)
        junkd = junkb
        stK = ExitStack()
        es.enter_context(stK)
        tri = sb("tri", [64, 256], F32, stK)
        cs = sb("cs", [128, 17 * 16], F32, stK)
        sublnb = sb("sublnb", [128, 128], F32, stK)
        glanb = sb("glanb", [128, 128], F32, stK)
        lam_t = sb("lam_t", [128, 256], F32, stK)
        lam_s = sb("lam_s", [128, 8], F32, stK)
        w2c = sb("w2c", [32, 512], F32, stK)
        bc = sb("bc", [1, 512], F32, stK)
        ones_row = sb("ones_row", [1, 128], F32, stK)
        ln1 = sb("ln1", [128, 8], F32, stK)

        def ld(eng, dst, src, key):
            P.dma(eng, I("dma_start", out=dst, in_=src), [], [key])

        ld("sp", identb[:], c_identb, "identb")
        ld("sp", identf[:], c_identf, "identf")
        ld("sp", tri[:], c_tri, "tri")
        ld("sp", cs[:], c_cs, "cs")
        ld("sp", iota[:], c_iota, "iota")
        ld("act", sublnb[:], subln_b, "sublnb")
        ld("act", glanb[:], glan_b, "glanb")
        ld("act", lam_t[:], lamv, "lam_t")
        ld("act", w2c[:], w2cat, "w2c")
        ld("act", bc[:], bcat, "bc")
        ld("act", ln1[:], ln1c, "ln1")
        ld("act", ln2[:], ln2c, "ln2")
        P.op("dve", I("memset", ones_row[:], 1.0), [], ["ones_row"])
        triF = tri[:, 0:64]
        triB = tri[:, 64:128]
        ones64 = tri[:, 128:192]
        onescol = tri[:, 192:193]

        P.op("dve", I("scalar_tensor_tensor", out=junkb[:, 0:64], in0=lam_t[:, 0:64], scalar=1.0, in1=lam_t[:, 64:128],
                                                    op0=ALU.mult, op1=ALU.mult, accum_out=lam_s[:, 0:1]), ["lam_t"], ["junkb", "lam_s"])
        P.op("dve", I("scalar_tensor_tensor", out=junkb[:, 0:64], in0=lam_t[:, 128:192], scalar=1.0, in1=lam_t[:, 192:256],
                                                    op0=ALU.mult, op1=ALU.mult, accum_out=lam_s[:, 1:2]), ["lam_t", "junkb"], ["junkb", "lam_s"])
        P.op("act", I("activation", out=lam_s[:, 2:4], in_=lam_s[:, 0:2], func=AF.Exp), ["lam_s"], ["lam_s"])
        P.op("dve", I("scalar_tensor_tensor", out=lam_s[:, 4:5], in0=lam_s[:, 3:4], scalar=-0.2, in1=lam_s[:, 2:3],
                                                    op0=ALU.add, op1=ALU.subtract), ["lam_s"], ["lam_s"])
        nlam = lam_s[:, 4:5]
        P.op("dve", I("tensor_scalar", out=sublnb[:], in0=sublnb[:], scalar1=0.8, scalar2=None, op0=ALU.mult), ["sublnb"], ["sublnb"])

        def rms(src_ap, Pn, width, skey, col, srckey):
            P.op("dve", I("scalar_tensor_tensor", out=junkd[0:Pn, 0:width], in0=src_ap, scalar=1.0, in1=src_ap, op0=ALU.mult, op1=ALU.mult,
                          accum_out=stat[0:Pn, col:col + 1]), [srckey], ["junkd", skey])
            P.op("act", I("activation", out=stat[0:Pn, col + 1:col + 2], in_=stat[0:Pn, col:col + 1], func=AF.Ln, bias=EPS, scale=1.0 / width), [skey], [skey])
            P.op("act", I("activation", out=stat[0:Pn, col + 2:col + 3], in_=stat[0:Pn, col + 1:col + 2], func=AF.Exp, scale=-0.5), [skey], [skey])
            return stat[0:Pn, col + 2:col + 3]

        st1 = ExitStack()
        stK.enter_context(st1)
        w_bf = sb("w_bf", [128, 8, IN_W], BF16, st1)
        st1b = ExitStack()
        st1.enter_context(st1b)
        qkT = sb("qkT", [128, 12, 2064], BF16, st1b)
        vtok = sb("vtok", [128, 17, 4, 129], BF16, st1b)
        stA = ExitStack()
        st1b.enter_context(stA)
        wst = [sb("wst%d" % i, [128, IN_W], F32, stA) for i in range(2)]
        for c in range(8):
            s = c % 2
            P.dma("sp" if s == 0 else "act", I("dma_start", out=wst[s][:], in_=w_in[c * 128:(c + 1) * 128, :]), [], ["wst%d" % s])
            if s == 0:
                P.op("dve", I("tensor_scalar", out=w_bf[:, c, :], in0=wst[s][:], scalar1=ln1[:, c:c + 1], scalar2=None, op0=ALU.mult),
                     ["wst%d" % s, "ln1"], ["w_bf"])
            else:
                P.op("act", I("activation", out=w_bf[:, c, :], in_=wst[s][:], func=AF.Copy, scale=ln1[:, c:c + 1]), ["wst%d" % s, "ln1"], ["w_bf"])
        P.op("dve", I("memset", vtok[:, :, :, 128:129], 1.0), [], ["vtok"])
        P.op("dve", I("memset", qkT[64:128, 0:4, :], 0.0), [], ["qkT"])
        P.op("dve", I("memset", qkT[0:64, 8:12, :], 0.0), [], ["qkT"])
        stA.close()
        P.barrier()

        stB = ExitStack()
        st1b.enter_context(stB)
        xt = [sb("xt%d" % i, [128, D], F32, stB) for i in range(2)]
        xs = [sb("xs%d" % i, [128, D], BF16, stB) for i in range(2)]
        xT = [sb("xT%d" % i, [128, 8, 128], BF16, stB) for i in range(2)]
        qk_sb = [sb("qk_sb%d" % i, [128, 1024], F32, stB) for i in range(2)]
        qk_bf = [sb("qk_bf%d" % i, [128, 1024], BF16, stB) for i in range(2)]
        rp = [sb("rp%d" % i, [128, 4, 16, 8], F32, stB) for i in range(2)]

        def load_norm_T(t_rows_ap, Pn, s, statcol, psbank):
            P.dma("sp", I("dma_start", out=xt[s][0:Pn, :], in_=t_rows_ap), [], ["xt%d" % s])
            r = rms(xt[s][0:Pn, :], Pn, D, "stat%d" % statcol, statcol, "xt%d" % s)
            P.op("dve", I("tensor_scalar", out=xs[s][0:Pn, :], in0=xt[s][0:Pn, :], scalar1=r, scalar2=None, op0=ALU.mult),
                 ["xt%d" % s, "stat%d" % statcol], ["xs%d" % s])
            pT = psb[psbank][:].bitcast(BF16)
            for k in range(8):
                P.op("pe", I("transpose", out=pT[:, k * 128:k * 128 + Pn], in_=xs[s][0:Pn, k * 128:(k + 1) * 128], identity=identb[0:Pn, 0:Pn]),
                     ["xs%d" % s, "identb"], [PSK(psbank)], inc=(k == 7))
            P.op("act", I("activation", out=xT[s][:, :, 0:Pn], in_=pT.rearrange("p (k n) -> p k n", k=8)[:, :, 0:Pn], func=AF.Copy),
                 [PSK(psbank)], ["xT%d" % s])

        A_lists = []
        for t in range(17):
            if dbg is None:
                P.rec = []
            s = t % 2
            Pn = 128 if t < 16 else 16
            rows = x[t * 128:(t + 1) * 128, :] if t < 16 else meta
            tok0 = t * 128
            load_norm_T(rows, Pn, s, (t % 2) * 4, 4 + (t % 2))
            for g in range(3):
                for k in range(8):
                    P.op("pe", I("matmul", psb[g][0:Pn, :], lhsT=xT[s][:, k, 0:Pn], rhs=w_bf[:, k, g * 512:(g + 1) * 512],
                                                           start=(k == 0), stop=(k == 7)),
                         ["xT%d" % s, "w_bf"], [PSK(g)], inc=(k == 7))
            P.op("act", I("activation", out=qk_sb[s][0:Pn, 0:512], in_=psb[0][0:Pn, :], func=AF.Copy, scale=0.125), [PSK(0)], ["qk_sb%d" % s])
            P.op("act", I("activation", out=qk_sb[s][0:Pn, 512:1024], in_=psb[1][0:Pn, :], func=AF.Copy), [PSK(1)], ["qk_sb%d" % s])
            P.op("dve", I("tensor_copy", out=vtok[0:Pn, t, :, 0:128], in_=psb[2][0:Pn, :].rearrange("p (h d) -> p h d", h=4)), [PSK(2)], ["vtok"])

            if dbg is not None and dbg[0] == "A1" and t == dbg[2]:
                dt_ = sb("dbgt", [128, 4096], F32)
                P.op("dve", I("memset", dt_[:], 0.0), [], ["dbgt"])
                P.op("dve", I("tensor_copy", out=dt_[0:Pn, 0:1024], in_=xs[s][0:Pn, :]), ["xs%d" % s], ["dbgt"])
                P.op("dve", I("tensor_copy", out=dt_[:, 1024:2048], in_=w_bf[:, 1, 0:1024]), ["w_bf"], ["dbgt"])
                P.op("dve", I("tensor_copy", out=dt_[:, 2048:3072].rearrange("p (k n) -> p k n", k=8), in_=xT[s][:, :, :]), ["xT%d" % s], ["dbgt"])
                P.op("dve", I("tensor_copy", out=dt_[0:Pn, 3072:4096], in_=qk_sb[s][0:Pn, :]), ["qk_sb%d" % s], ["dbgt"])
                P.dma("sp", I("dma_start", out=dbg_out, in_=dt_[:]), ["dbgt"], [], final=True)
                P.emit()
                return nc
            qv = qk_sb[s][0:Pn, :].rearrange("p (g d) -> p g d", g=16)
            x1 = qv[:, :, 0:8]
            x2 = qv[:, :, 8:16]
            cosb = cs[0:Pn, t * 16:t * 16 + 8].unsqueeze(1).broadcast_to([Pn, 16, 8])
            sinb = cs[0:Pn, t * 16 + 8:t * 16 + 16].unsqueeze(1).broadcast_to([Pn, 16, 8])
            rk = "rp%d" % s
            P.op("pool", I("tensor_tensor", out=rp[s][0:Pn, 0], in0=x1, in1=cosb, op=ALU.mult), ["qk_sb%d" % s, "cs"], [rk])
            P.op("pool", I("tensor_tensor", out=rp[s][0:Pn, 1], in0=x2, in1=sinb, op=ALU.mult), ["qk_sb%d" % s, "cs"], [rk])
            P.op("pool", I("tensor_tensor", out=rp[s][0:Pn, 2], in0=x2, in1=cosb, op=ALU.mult), ["qk_sb%d" % s, "cs"], [rk])
            P.op("pool", I("tensor_tensor", out=rp[s][0:Pn, 3], in0=x1, in1=sinb, op=ALU.mult), ["qk_sb%d" % s, "cs"], [rk])
            P.op("dve", I("tensor_copy", out=qk_bf[s][0:Pn, :], in_=qk_sb[s][0:Pn, :]), ["qk_sb%d" % s], ["qk_bf%d" % s])
            qbv = qk_bf[s][0:Pn, :].rearrange("p (g d) -> p g d", g=16)
            P.op("dve", I("tensor_tensor", out=qbv[:, :, 0:8], in0=rp[s][0:Pn, 0], in1=rp[s][0:Pn, 1], op=ALU.subtract), [rk], ["qk_bf%d" % s])
            P.op("dve", I("tensor_tensor", out=qbv[:, :, 8:16], in0=rp[s][0:Pn, 2], in1=rp[s][0:Pn, 3], op=ALU.add), [rk], ["qk_bf%d" % s])
            bank = 6 + (t % 2)
            pT = psb[bank][:].bitcast(BF16)
            for j in range(8):
                P.op("pe", I("transpose", out=pT[:, j * 128:j * 128 + Pn], in_=qk_bf[s][0:Pn, j * 128:(j + 1) * 128], identity=identb[0:Pn, 0:Pn]),
                     ["qk_bf%d" % s, "identb"], [PSK(bank)], inc=(j == 7))
            pT3 = pT.rearrange("p (k n) -> p k n", k=8)
            P.op("act", I("activation", out=qkT[0:64, 0:4, tok0:tok0 + Pn], in_=pT3[0:64, 0:4, 0:Pn], func=AF.Copy), [PSK(bank)], ["qkT"])
            P.op("dve", I("tensor_copy", out=qkT[64:128, 8:12, tok0:tok0 + Pn], in_=pT3[64:128, 0:4, 0:Pn]), [PSK(bank)], ["qkT"])
            P.op("act", I("activation", out=qkT[:, 4:8, tok0:tok0 + Pn], in_=pT3[:, 4:8, 0:Pn], func=AF.Copy), [PSK(bank)], ["qkT"])
            if dbg is None:
                A_lists.append(P.rec)
                P.rec = None

            if dbg is not None and dbg[0] == "A2" and t == dbg[2]:
                dt_ = sb("dbgt", [128, 4096], F32)
                P.op("dve", I("memset", dt_[:], 0.0), [], ["dbgt"])
                P.op("dve", I("tensor_copy", out=dt_[0:Pn, 0:1024], in_=qk_bf[s][0:Pn, :]), ["qk_bf%d" % s], ["dbgt"])
                P.op("dve", I("tensor_copy", out=dt_[:, 1024:2048].rearrange("p (k n) -> p k n", k=8)[:, :, 0:Pn], in_=qkT[:, :, tok0:tok0 + Pn]), ["qkT"], ["dbgt"])
                P.op("dve", I("tensor_copy", out=dt_[:, 2048:3072], in_=pT), [PSK(bank)], ["dbgt"])
                for j_ in range(8):
                    P.op("dve", I("tensor_copy", out=dt_[:, 3072 + j_ * 128:3072 + (j_ + 1) * 128], in_=qkT[:, j_, 0:128]), ["qkT"], ["dbgt"])
                P.dma("sp", I("dma_start", out=dbg_out, in_=dt_[:]), ["dbgt"], [], final=True)
                P.emit()
                return nc

        if dbg is None:
            cur = A_lists[0]
            ci = 0
            while ci < len(cur) // 2:
                ci = rgroup(P, cur, ci)
            for n_ in range(len(A_lists)):
                cur = A_lists[n_]
                nxt = A_lists[n_ + 1] if n_ + 1 < len(A_lists) else []
                half_n = len(nxt) // 2
                ni = 0
                while ci < len(cur) or ni < half_n:
                    if ci < len(cur):
                        ci = rgroup(P, cur, ci)
                    if ni < half_n:
                        ni = rgroup(P, nxt, ni)
                ci = ni
        if dbg is not None and dbg[0] == "A":
            dt_ = sb("dbgt", [128, 2064], F32)
            for tt in range(17):
                n_ = 128 if tt < 16 else 16
                P.op("dve", I("tensor_copy", out=dt_[:, tt * 128:tt * 128 + n_], in_=qkT[:, dbg[2], tt * 128:tt * 128 + n_]), ["qkT"], ["dbgt"])
            P.dma("sp", I("dma_start", out=dbg_out, in_=dt_[:]), ["dbgt"], [], final=True)
            P.emit()
            return nc
        stB.close()
        P.barrier()

        stC = ExitStack()
        st1b.enter_context(stC)
        eT_all = sb("eT_all", [128, 17, 512], BF16, stC)
        t0b = [sb("t0b%d" % i, [128, 128], F32, stC) for i in range(2)]
        dab = [sb("dab%d" % i, [128, 128], F32, stC) for i in range(2)]
        rz = [sb("rz%d" % i, [128, 8], F32, stC) for i in range(2)]
        dstat = sb("dstat", [128, 192], F32, stC)
        eTs = [(eT_all, "eT_all"), (og_full[:, 0:17, :], "og")]
        combos = [(qb, h) for qb in range(8) for h in range(4)]
        it_c = [0]
        fin_c = [0]

        def emit_S(i, kts):
            qb, h = combos[i]
            q0 = qb * 256
            eb, ek = eTs[i % 2]
            for kt in kts:
                Pk = 128 if kt < 16 else 16
                k0 = kt * 128
                sbk = it_c[0] % 3
                it_c[0] += 1
                for c in range(2):
                    P.op("pe", I("matmul", psb[sbk][0:Pk, c * 256:(c + 1) * 256], lhsT=qkT[:, 4 + h, k0:k0 + Pk],
                                 rhs=qkT[:, (0 if c == 0 else 8) + h, q0:q0 + 256], start=True, stop=True),
                         ["qkT"], [PSK(sbk)], inc=(c == 1))
                P.op("act", I("activation", out=eb[0:Pk, kt, :], in_=psb[sbk][0:Pk, :], func=AF.Exp), [PSK(sbk)], [ek])

        def emit_PV(i, g):
            qb, h = combos[i]
            eb, ek = eTs[i % 2]
            c, qs = g // 2, g % 2
            bank = 4 + g
            for kt in range(17):
                Pk = 128 if kt < 16 else 16
                P.op("pe", I("matmul", psb[bank][:, 0:129], lhsT=eb[0:Pk, kt, c * 256 + qs * 128:c * 256 + (qs + 1) * 128],
                             rhs=vtok[0:Pk, kt, h, :], start=(kt == 0), stop=(kt == 16)),
                     [ek, "vtok"], [PSK(bank)], inc=(kt == 16))

        def emit_fin(i):
            qb, h = combos[i]
            for qs in range(2):
                f = fin_c[0] % 2
                fin_c[0] += 1
                tile_i = qb * 2 + qs
                ab = 4 + qs
                a0 = psb[4 + qs][:, 0:129]
                a1 = psb[6 + qs][:, 0:129]
                rk = "rz%d" % f
                P.op("dve", I("reciprocal", out=rz[f][:, 0:1], in_=a0[:, 128:129]), [PSK(ab)], [rk])
                P.op("dve", I("reciprocal", out=rz[f][:, 1:2], in_=a1[:, 128:129]), [PSK(ab + 2)], [rk])
                P.op("dve", I("tensor_tensor", out=rz[f][:, 2:3], in0=rz[f][:, 1:2], in1=nlam, op=ALU.mult), [rk, "lam_s"], [rk])
                P.op("dve", I("tensor_scalar", out=t0b[f][:], in0=a0[:, 0:128], scalar1=rz[f][:, 0:1], scalar2=None, op0=ALU.mult),
                     [PSK(ab), rk], ["t0b%d" % f])
                P.op("dve", I("scalar_tensor_tensor", out=dab[f][:], in0=a1[:, 0:128], scalar=rz[f][:, 2:3], in1=t0b[f][:],
                              op0=ALU.mult, op1=ALU.add), [PSK(ab + 2), rk, "t0b%d" % f], ["dab%d" % f])
                sidx = tile_i * 4 + h
                P.op("dve", I("scalar_tensor_tensor", out=junkd[:, 0:128], in0=dab[f][:], scalar=1.0, in1=dab[f][:], op0=ALU.mult, op1=ALU.mult,
                              accum_out=dstat[:, sidx:sidx + 1]), ["dab%d" % f], ["junkd", "dstat"])
                P.op("dve", I("tensor_tensor", out=mix_da[:, tile_i, h * 128:(h + 1) * 128], in0=dab[f][:], in1=sublnb[:], op=ALU.mult),
                     ["dab%d" % f, "sublnb"], ["mix_da"])

        kt_parts = [list(range(0, 5)), list(range(5, 9)), list(range(9, 13)), list(range(13, 17))]
        emit_S(0, list(range(17)))
        for i in range(32):
            for g in range(4):
                emit_PV(i, g)
                if i + 1 < 32:
                    emit_S(i + 1, kt_parts[g])
            emit_fin(i)
        P.op("act", I("activation", out=dstat[:, 64:128], in_=dstat[:, 0:64], func=AF.Ln, bias=EPS, scale=1.0 / 128), ["dstat"], ["dstat"])
        P.op("act", I("activation", out=dstat[:, 128:192], in_=dstat[:, 64:128], func=AF.Exp, scale=-0.5), ["dstat"], ["dstat"])
        m4 = mix_da[:, :, :].rearrange("p t (h d) -> p (t h) d", h=4)
        P.op("dve", I("tensor_tensor", out=m4, in0=m4, in1=dstat[:, 128:192].unsqueeze(2).broadcast_to([128, 64, 128]), op=ALU.mult), ["mix_da", "dstat"], ["mix_da"])
        stC.close()
        if dbg is not None and dbg[0] == "B":
            dt_ = sb("dbgt", [128, 16 * 512], F32)
            P.op("dve", I("tensor_copy", out=dt_[:], in_=mix_da[:].rearrange("p t c -> p (t c)")), ["mix_da"], ["dbgt"])
            P.dma("sp", I("dma_start", out=dbg_out, in_=dt_[:]), ["dbgt"], [], final=True)
            P.emit()
            return nc
        st1b.close()
        P.barrier()

        stG = ExitStack()
        st1.enter_context(stG)
        xc_2 = [sb("xc%d" % i, [64, D], F32, stG) for i in range(2)]
        xcs_2 = [sb("xcs%d" % i, [64, D], BF16, stG) for i in range(2)]
        xTc_2 = [sb("xTc%d" % i, [128, 8, 64], BF16, stG) for i in range(2)]
        gqk_2 = [sb("gqk%d" % i, [64, 512], F32, stG) for i in range(2)]
        gv_bf_2 = [sb("gv_bf%d" % i, [64, 512], BF16, stG) for i in range(2)]
        sgr_2 = [sb("sgr%d" % i, [64, 512], BF16, stG) for i in range(2)]
        z_sb_2 = [sb("z_sb%d" % i, [64, 32], F32, stG) for i in range(2)]
        zT_sb_2 = [sb("zT_sb%d" % i, [32, 64], F32, stG) for i in range(2)]
        e_sb_2 = [sb("e_sb%d" % i, [64, 256], F32, stG) for i in range(2)]
        l_sb_2 = [sb("l_sb%d" % i, [64, 256], F32, stG) for i in range(2)]
        Lc_sb_2 = [sb("Lc_sb%d" % i, [64, 256], F32, stG) for i in range(2)]
        Dm_sb_2 = [sb("Dm_sb%d" % i, [64, 256], F32, stG) for i in range(2)]
        Eq_2 = [sb("Eq%d" % i, [64, 256], F32, stG) for i in range(2)]
        Ek_2 = [sb("Ek%d" % i, [64, 256], F32, stG) for i in range(2)]
        Eh_2 = [sb("Eh%d" % i, [64, 256], F32, stG) for i in range(2)]
        qt_bf_2 = [sb("qt_bf%d" % i, [64, 256], BF16, stG) for i in range(2)]
        kt_bf_2 = [sb("kt_bf%d" % i, [64, 256], BF16, stG) for i in range(2)]
        kh_bf_2 = [sb("kh_bf%d" % i, [64, 256], BF16, stG) for i in range(2)]
        qkTc_2 = [sb("qkTc%d" % i, [64, 8, 64], BF16, stG) for i in range(2)]
        attn_bf_2 = [sb("attn_bf%d" % i, [64, 4, 64], BF16, stG) for i in range(2)]
        S_f = sb("S_f", [64, 4, 128], F32, stG)
        S_bf = sb("S_bf", [64, 4, 128], BF16, stG)
        dec_sb_2 = [sb("dec_sb%d" % i, [64, 4], F32, stG) for i in range(2)]
        tot_2 = [sb("tot%d" % i, [64, 512], F32, stG) for i in range(2)]
        sq_2 = [sb("sq%d" % i, [64, 512], F32, stG) for i in range(2)]
        ssg_2 = [sb("ssg%d" % i, [64, 16], F32, stG) for i in range(2)]
        cst = [sb("cst%d" % i, [128, 2 * D], F32, stG) for i in range(2)]
        cbf = [sb("cbf%d" % i, [128, 2 * D], BF16, stG) for i in range(2)]
        conv_state = [0]
        gla_step = [0]

        NBLK = 144

        def conv_src(blk):
            if blk < 128:
                return peer_uv[blk * 128:(blk + 1) * 128, :], uv_bf[blk * 128:(blk + 1) * 128, :], 2 * D, None, []
            if blk < 136:
                c_ = blk - 128
                return w_q[c_ * 128:(c_ + 1) * 128, :], wq_dram[c_ * 128:(c_ + 1) * 128, :], 2 * D, ln2[:, c_:c_ + 1], ["wq_dram"]
            c_ = blk - 136
            return w_out[c_ * 128:(c_ + 1) * 128, :], wo_dram[c_ * 128:(c_ + 1) * 128, :], D, None, ["wo_dram"]

        def conv_finish(blk):
            s_ = blk % 2
            src, dst, wd, scl, wk = conv_src(blk)
            if scl is None and blk % 2 == 1:
                P.op("dve", I("tensor_copy", out=cbf[s_][:, 0:wd], in_=cst[s_][:, 0:wd]), ["cst%d" % s_], ["cbf%d" % s_])
            elif scl is None:
                P.op("act", I("activation", out=cbf[s_][:, 0:wd], in_=cst[s_][:, 0:wd], func=AF.Copy), ["cst%d" % s_], ["cbf%d" % s_])
            else:
                P.op("act", I("activation", out=cbf[s_][:, 0:wd], in_=cst[s_][:, 0:wd], func=AF.Copy, scale=scl), ["cst%d" % s_, "ln2"], ["cbf%d" % s_])
            P.dma("pool", I("dma_start", out=dst, in_=cbf[s_][:, 0:wd]), ["cbf%d" % s_], wk)

        def conv_blocks(n):
            for _ in range(n):
                blk = conv_state[0]
                if blk > NBLK:
                    return
                conv_state[0] += 1
                if blk < NBLK:
                    s_ = blk % 2
                    src, dst, wd, scl, wk = conv_src(blk)
                    P.dma("pool", I("dma_start", out=cst[s_][:, 0:wd], in_=src), [], ["cst%d" % s_])
                if blk >= 1:
                    conv_finish(blk - 1)

        def gla_chunk(c, dirn, first):
            pp = gla_step[0] % 2
            gla_step[0] += 1
            xc = xc_2[pp]
            xcs = xcs_2[pp]
            xTc = xTc_2[pp]
            gqk = gqk_2[pp]
            gv_bf = gv_bf_2[pp]
            sgr = sgr_2[pp]
            z_sb = z_sb_2[pp]
            zT_sb = zT_sb_2[pp]
            e_sb = e_sb_2[pp]
            l_sb = l_sb_2[pp]
            Lc_sb = Lc_sb_2[pp]
            Dm_sb = Dm_sb_2[pp]
            Eq = Eq_2[pp]
            Ek = Ek_2[pp]
            Eh = Eh_2[pp]
            qt_bf = qt_bf_2[pp]
            kt_bf = kt_bf_2[pp]
            kh_bf = kh_bf_2[pp]
            qkTc = qkTc_2[pp]
            attn_bf = attn_bf_2[pp]
            dec_sb = dec_sb_2[pp]
            tot = tot_2[pp]
            sq = sq_2[pp]
            ssg = ssg_2[pp]
            Pc = 16 if c == 0 else 64
            rows = meta if c == 0 else x[(c - 1) * 64:c * 64, :]
            need_o = (c >= 1)
            P.dma("sp", I("dma_start", out=xc[0:Pc, :], in_=rows), [], [("xc%d" % pp)])
            r = rms(xc[0:Pc, :], Pc, D, "statG%d" % pp, 4 * pp, ("xc%d" % pp))
            P.op("dve", I("tensor_scalar", out=xcs[0:Pc, :], in0=xc[0:Pc, :], scalar1=r, scalar2=None, op0=ALU.mult), [("xc%d" % pp), "statG%d" % pp], [("xcs%d" % pp)])
            pT = psb[0][:].bitcast(BF16)
            for k in range(8):
                P.op("pe", I("transpose", out=pT[:, k * 64:k * 64 + Pc], in_=xcs[0:Pc, k * 128:(k + 1) * 128], identity=identb[0:Pc, 0:Pc]),
                     [("xcs%d" % pp), "identb"], [PSK(0)], inc=(k == 7))
            P.op("act", I("activation", out=xTc[:, :, 0:Pc], in_=pT[:, 0:512].rearrange("p (k n) -> p k n", k=8)[:, :, 0:Pc], func=AF.Copy), [PSK(0)], [("xTc%d" % pp)])
            groups = [(1, 1536, 512), (2, 2048, 512), (4, 3072, 32)]
            if dirn == 1:
                groups.append((3, 2560, 512))
            for bank, c0, ncol in groups:
                for k in range(8):
                    P.op("pe", I("matmul", psb[bank][0:Pc, 0:ncol], lhsT=xTc[:, k, 0:Pc], rhs=w_bf[:, k, c0:c0 + ncol], start=(k == 0), stop=(k == 7)),
                         [("xTc%d" % pp), "w_bf"], [PSK(bank)], inc=(k == 7))
            P.op("act", I("activation", out=gqk[0:Pc, 0:256], in_=psb[1][0:Pc, 0:256], func=AF.Copy, scale=0.125), [PSK(1)], [("gqk%d" % pp)])
            P.op("act", I("activation", out=gqk[0:Pc, 256:512], in_=psb[1][0:Pc, 256:512], func=AF.Copy), [PSK(1)], [("gqk%d" % pp)])
            P.op("act", I("activation", out=gv_bf[0:Pc, :], in_=psb[2][0:Pc, :], func=AF.Copy), [PSK(2)], [("gv_bf%d" % pp)])
            P.op("dve", I("tensor_copy", out=z_sb[0:Pc, :], in_=psb[4][0:Pc, 0:32]), [PSK(4)], [("z_sb%d" % pp)])
            if dirn == 1:
                P.op("act", I("activation", out=sq[0:Pc, :], in_=psb[3][0:Pc, :], func=AF.Exp, scale=-1.0), [PSK(3)], [("sq%d" % pp)])
                P.op("act", I("activation", out=sq[0:Pc, :], in_=sq[0:Pc, :], func=AF.Ln, bias=1.0), [("sq%d" % pp)], [("sq%d" % pp)])
                P.op("act", I("activation", out=sq[0:Pc, :], in_=sq[0:Pc, :], func=AF.Exp, scale=-1.0), [("sq%d" % pp)], [("sq%d" % pp)])
                P.op("dve", I("tensor_tensor", out=sgr[0:Pc, :], in0=psb[3][0:Pc, :], in1=sq[0:Pc, :], op=ALU.mult), [PSK(3), ("sq%d" % pp)], [("sgr%d" % pp)])
            P.op("pe", I("transpose", out=psb[4][0:32, 32:32 + Pc], in_=z_sb[0:Pc, 0:32], identity=identf[0:Pc, 0:Pc]), [("z_sb%d" % pp), "identf"], [PSK(4)])
            P.op("dve", I("tensor_copy", out=zT_sb[:, 0:Pc], in_=psb[4][0:32, 32:32 + Pc]), [PSK(4)], [("zT_sb%d" % pp)])
            g0 = dirn * 256
            P.op("pe", I("matmul", psb[5][0:Pc, 0:256], lhsT=zT_sb[:, 0:Pc], rhs=w2c[:, g0:g0 + 256], start=True, stop=False), [("zT_sb%d" % pp), "w2c"], [PSK(5)], inc=False)
            P.op("pe", I("matmul", psb[5][0:Pc, 0:256], lhsT=ones_row[0:1, 0:Pc], rhs=bc[0:1, g0:g0 + 256], start=False, stop=True), ["ones_row", "bc"], [PSK(5)])
            P.op("act", I("activation", out=e_sb[0:Pc, :], in_=psb[5][0:Pc, 0:256], func=AF.Exp, scale=-1.0), [PSK(5)], [("e_sb%d" % pp)])
            P.op("act", I("activation", out=l_sb[0:Pc, :], in_=e_sb[0:Pc, :], func=AF.Ln, bias=1.0), [("e_sb%d" % pp)], [("l_sb%d" % pp)])
            triD = triF if dirn == 0 else triB
            P.op("pe", I("matmul", psb[6][0:Pc, 0:256], lhsT=triD[0:Pc, 0:Pc], rhs=l_sb[0:Pc, :], start=True, stop=True), ["tri", ("l_sb%d" % pp)], [PSK(6)], inc=False)
            P.op("pe", I("matmul", psb[6][0:Pc, 256:512], lhsT=ones64[0:Pc, 0:Pc], rhs=l_sb[0:Pc, :], start=True, stop=True), ["tri", ("l_sb%d" % pp)], [PSK(6)])
            for hh in range(4):
                P.op("pe", I("matmul", psb[4][0:64, 96 + hh:97 + hh], lhsT=l_sb[0:Pc, hh * 64:(hh + 1) * 64], rhs=onescol[0:Pc, 0:1], start=True, stop=True),
                     [("l_sb%d" % pp), "tri"], [PSK(4)], inc=(hh == 3))
            P.op("act", I("activation", out=dec_sb[:, :], in_=psb[4][0:64, 96:100], func=AF.Exp, scale=-1.0 / 16), [PSK(4)], [("dec_sb%d" % pp)])
            if need_o:
                P.op("act", I("activation", out=Eq[0:Pc, :], in_=psb[6][0:Pc, 0:256], func=AF.Exp, scale=-1.0 / 16), [PSK(6)], [("Eq%d" % pp)])
                P.op("act", I("activation", out=Ek[0:Pc, :], in_=psb[6][0:Pc, 0:256], func=AF.Exp, scale=1.0 / 16), [PSK(6)], [("Ek%d" % pp)])
            P.op("act", I("activation", out=Lc_sb[0:Pc, :], in_=psb[6][0:Pc, 0:256], func=AF.Copy), [PSK(6)], [("Lc_sb%d" % pp)])
            P.op("dve", I("tensor_tensor", out=Dm_sb[0:Pc, :], in0=psb[6][0:Pc, 256:512], in1=Lc_sb[0:Pc, :], op=ALU.subtract), [PSK(6), ("Lc_sb%d" % pp)], [("Dm_sb%d" % pp)])
            P.op("act", I("activation", out=Eh[0:Pc, :], in_=Dm_sb[0:Pc, :], func=AF.Exp, scale=-1.0 / 16), [("Dm_sb%d" % pp)], [("Eh%d" % pp)])
            P.op("dve", I("tensor_tensor", out=kh_bf[0:Pc, :], in0=gqk[0:Pc, 256:512], in1=Eh[0:Pc, :], op=ALU.mult), [("gqk%d" % pp), ("Eh%d" % pp)], [("kh_bf%d" % pp)])
            if need_o:
                P.op("dve", I("tensor_tensor", out=qt_bf[0:Pc, :], in0=gqk[0:Pc, 0:256], in1=Eq[0:Pc, :], op=ALU.mult), [("gqk%d" % pp), ("Eq%d" % pp)], [("qt_bf%d" % pp)])
                P.op("dve", I("tensor_tensor", out=kt_bf[0:Pc, :], in0=gqk[0:Pc, 256:512], in1=Ek[0:Pc, :], op=ALU.mult), [("gqk%d" % pp), ("Ek%d" % pp)], [("kt_bf%d" % pp)])
                p7 = psb[7][:].bitcast(BF16)
                for hh in range(4):
                    P.op("pe", I("transpose", out=p7[0:64, hh * 64:hh * 64 + Pc], in_=qt_bf[0:Pc, hh * 64:(hh + 1) * 64], identity=identb[0:Pc, 0:Pc]),
                         [("qt_bf%d" % pp), "identb"], [PSK(7)], inc=False)
                for hh in range(4):
                    P.op("pe", I("transpose", out=p7[0:64, (4 + hh) * 64:(4 + hh) * 64 + Pc], in_=kt_bf[0:Pc, hh * 64:(hh + 1) * 64], identity=identb[0:Pc, 0:Pc]),
                         [("kt_bf%d" % pp), "identb"], [PSK(7)], inc=(hh == 3))
                P.op("act", I("activation", out=qkTc[:, :, 0:Pc], in_=p7[0:64, 0:512].rearrange("p (k n) -> p k n", k=8)[:, :, 0:Pc], func=AF.Copy), [PSK(7)], [("qkTc%d" % pp)])
                psA = psb[7][0:64, 256:512].rearrange("p (h n) -> p h n", h=4)
                for hh in range(4):
                    P.op("pe", I("matmul", psA[0:Pc, hh, 0:Pc], lhsT=qkTc[:, 4 + hh, 0:Pc], rhs=qkTc[:, hh, 0:Pc], start=True, stop=True),
                         [("qkTc%d" % pp)], [PSK(7)], inc=(hh == 3))
                P.op("dve", I("tensor_tensor", out=attn_bf[0:Pc, :, 0:Pc], in0=psA[0:Pc, :, 0:Pc], in1=triD[0:Pc, 0:Pc].unsqueeze(1).broadcast_to([Pc, 4, Pc]), op=ALU.mult),
                     [PSK(7), "tri"], [("attn_bf%d" % pp)])
                psO = psb[1][0:64, :].rearrange("p (h n) -> p h n", h=4)
                for hh in range(4):
                    P.op("pe", I("matmul", psO[0:Pc, hh, :], lhsT=attn_bf[0:Pc, hh, 0:Pc], rhs=gv_bf[0:Pc, hh * 128:(hh + 1) * 128], start=True, stop=first),
                         [("attn_bf%d" % pp), ("gv_bf%d" % pp)], [PSK(1)], inc=(first and hh == 3))
                    if not first:
                        P.op("pe", I("matmul", psO[0:Pc, hh, :], lhsT=qkTc[:, hh, 0:Pc], rhs=S_bf[:, hh, :], start=False, stop=True),
                             [("qkTc%d" % pp), "S_bf"], [PSK(1)], inc=(hh == 3))
            psP = psb[2][0:64, :].rearrange("p (h n) -> p h n", h=4)
            for hh in range(4):
                P.op("pe", I("matmul", psP[:, hh, :], lhsT=kh_bf[0:Pc, hh * 64:(hh + 1) * 64], rhs=gv_bf[0:Pc, hh * 128:(hh + 1) * 128], start=True, stop=True),
                     [("kh_bf%d" % pp), ("gv_bf%d" % pp)], [PSK(2)], inc=(hh == 3))
            if need_o:
                if dirn == 0:
                    P.op("act", I("activation", out=og[0:64, c - 1, :], in_=psb[1][0:64, :], func=AF.Copy), [PSK(1)], ["og"])
                else:
                    P.op("dve", I("tensor_tensor", out=tot[:, :], in0=psb[1][0:64, :], in1=og[0:64, c - 1, :], op=ALU.add), [PSK(1), "og"], [("tot%d" % pp)])
                    P.op("dve", I("tensor_tensor", out=sq[:, :], in0=tot[:, :], in1=tot[:, :], op=ALU.mult), [("tot%d" % pp)], [("sq%d" % pp)])
                    P.op("dve", I("tensor_reduce", out=ssg[:, 0:4], in_=sq[:, :].rearrange("p (h n) -> p h n", h=4), axis=AX.X, op=ALU.add), [("sq%d" % pp)], [("ssg%d" % pp)])
                    P.op("act", I("activation", out=ssg[:, 4:8], in_=ssg[:, 0:4], func=AF.Ln, bias=EPS, scale=1.0 / 128), [("ssg%d" % pp)], [("ssg%d" % pp)])
                    P.op("act", I("activation", out=ssg[:, 8:12], in_=ssg[:, 4:8], func=AF.Exp, scale=-0.5), [("ssg%d" % pp)], [("ssg%d" % pp)])
                    t3 = tot[:, :].rearrange("p (h n) -> p h n", h=4)
                    P.op("dve", I("tensor_tensor", out=t3, in0=t3, in1=ssg[:, 8:12].unsqueeze(2).broadcast_to([64, 4, 128]), op=ALU.mult), [("tot%d" % pp), ("ssg%d" % pp)], [("tot%d" % pp)])
                    P.op("dve", I("tensor_tensor", out=t3, in0=t3, in1=glanb[0:64, :].unsqueeze(1).broadcast_to([64, 4, 128]), op=ALU.mult), [("tot%d" % pp), "glanb"], [("tot%d" % pp)])
                    P.op("dve", I("tensor_tensor", out=og[0:64, c - 1, :], in0=tot[:, :], in1=sgr[:, :], op=ALU.mult), [("tot%d" % pp), ("sgr%d" % pp)], ["og"])
            if first:
                P.op("dve", I("tensor_copy", out=S_f[:, :, :], in_=psP), [PSK(2)], ["S_f"])
            else:
                P.op("dve", I("tensor_tensor", out=S_f[:, :, :], in0=S_f[:, :, :], in1=dec_sb[:, :].unsqueeze(2).broadcast_to([64, 4, 128]), op=ALU.mult),
                     ["S_f", ("dec_sb%d" % pp)], ["S_f"])
                P.op("dve", I("tensor_tensor", out=S_f[:, :, :], in0=S_f[:, :, :], in1=psP, op=ALU.add), ["S_f", PSK(2)], ["S_f"])
            P.op("act", I("activation", out=S_bf[:, :, :], in_=S_f[:, :, :], func=AF.Copy), ["S_f"], ["S_bf"])

        sched = [(c, 0, c == 0) for c in range(0, 33)] + [(c, 1, c == 32) for c in range(32, 0, -1)]
        lists = []
        for (c, d_, fi) in sched:
            P.rec = []
            gla_chunk(c, d_, fi)
            lists.append(P.rec)
            P.rec = None

        def touches_state(r):
            return any(k in ("S_bf", "S_f") for k in r[3]) or any(k in ("S_bf", "S_f") for k in r[4])

        cur = lists[0]
        ci = 0
        for r in cur[:len(cur) // 2]:
            P.replay(r)
        ci = len(cur) // 2
        for n_ in range(len(lists)):
            cur = lists[n_]
            nxt = lists[n_ + 1] if n_ + 1 < len(lists) else []
            half_n = len(nxt) // 2
            ni = 0
            while ci < len(cur) or ni < half_n:
                if ci < len(cur):
                    P.replay(cur[ci])
                    ci += 1
                    while ci < len(cur) and cur[ci - 1][1] == "pe" and cur[ci - 1][5] is False:
                        P.replay(cur[ci])
                        ci += 1
                if ni < half_n:
                    if touches_state(nxt[ni]) and ci < len(cur):
                        continue
                    P.replay(nxt[ni])
                    ni += 1
                    while ni < half_n and nxt[ni - 1][1] == "pe" and nxt[ni - 1][5] is False:
                        P.replay(nxt[ni])
                        ni += 1
            ci = ni
            conv_blocks(3 if n_ < 16 else 2)
        conv_blocks(NBLK + 2)
        if dbg is not None and dbg[0] == "C":
            dt_ = sb("dbgt", [64, 32 * 512], F32)
            P.op("dve", I("tensor_copy", out=dt_[:], in_=og[:].rearrange("p t c -> p (t c)")), ["og"], ["dbgt"])
            P.dma("sp", I("dma_start", out=dbg_out, in_=dt_[:]), ["dbgt"], [], final=True)
            P.emit()
            return nc
        stG.close()
        st1.close()
        stK.close()
        P.barrier()

        keys_sb = sb("keys_sb", [128, 2048], F32)
        ln2b = sb("ln2b", [128, D], F32)
        finb = sb("finb", [128, D], F32)
        ld("act", keys_sb[:], keysT, "keys_sb")
        ld("act", ln2b[:], ln2_b, "ln2b")
        ld("act", finb[:], fin_b, "finb")
        wpiece = [0]
        wout_bf = sb("wout_bf", [128, 8, 1024], BF16)
        for nb in range(2):
            P.dma("sp", I("dma_start", out=wout_bf[:, :, nb * 512:(nb + 1) * 512], in_=wo_dram[:, nb * 512:(nb + 1) * 512].rearrange("(k p) c -> p k c", p=128)),
                  ["wo_dram"], ["wout_bf"])
        wqs = [sb("wqs%d" % i, [128, 8, 512], BF16) for i in range(2)]
        mixT = sb("mixT", [128, 8, 128], BF16)
        h2_2 = [sb("h2_%d" % i, [128, D], F32) for i in range(2)]
        hn_2 = [sb("hn_%d" % i, [128, D], BF16) for i in range(2)]
        hs_bf = sb("hs_bf", [128, D], BF16)
        hT = sb("hT", [128, 8, 128], BF16)
        qT_sb = sb("qT_sb", [128, 16, 128], F32)
        s_sb = sb("s_sb", [128, 16, 128], F32)
        ts = sb("ts", [128, 16, 16], F32)
        ti = sb("ti", [128, 16, 16], U32)
        tif = sb("tif", [128, 16, 16], F32)
        cand = sb("cand", [128, 2, 256], F32)
        bs = sb("bs", [128, 8, 16], F32)
        pos = sb("pos", [128, 8, 16], U32)
        idxf = sb("idxf", [128, 128], F32)
        idxi_2 = [sb("idxi_%d" % i, [128, 128], I32) for i in range(2)]
        gate_2 = [sb("gate_%d" % i, [128, 128], F32) for i in range(2)]
        prodb = [sb("prodb%d" % i, [128, D], BF16) for i in range(3)]
        gz = sb("gz", [128, 16], F32)
        apre = sb("apre", [128, 128], F32)
        gel = sb("gel", [128, 128], F32)
        s_bfv = s_sb[:, :, :].rearrange("p a b -> p (a b)").bitcast(BF16)
        prod = [s_bfv[:, i * 1024:(i + 1) * 1024] for i in range(4)]
        ai = sb("ai", [128, 128], U32)
        af = sb("af", [128, 2, 128], F32)
        rsel = sb("rsel", [128, 2, 128], F32)
        iota16 = iota[:, 0:16]
        diag = [sb("diag%d" % i, [128, 128], BF16) for i in range(4)]
        NG = 13
        gbuf = [sb("gbuf%d" % i, [128, 2 * D], BF16) for i in range(NG)]
        outsb = qT_sb[:, 0:8, :].rearrange("p a b -> p (a b)")
        gi = 0

        INTERLEAVE = 2
        L1 = []
        L1a = []
        L2 = []
        for t in range(16):
            tp = t % 2
            h2 = h2_2[tp]
            hn = hn_2[tp]
            idxi = idxi_2[tp]
            gate = gate_2[tp]
            P.rec = []
            pT = psb[0][:].bitcast(BF16)
            for k in range(4):
                P.op("pe", I("transpose", out=pT[:, k * 128:(k + 1) * 128], in_=mix_da[:, t, k * 128:(k + 1) * 128], identity=identb[:, :]),
                     ["mix_da", "identb"], [PSK(0)], inc=False)
            for half in range(2):
                for k in range(4):
                    P.op("pe", I("transpose", out=pT[:, (4 + k) * 128 + half * 64:(4 + k) * 128 + half * 64 + 64], in_=og[0:64, 2 * t + half, k * 128:(k + 1) * 128],
                                 identity=identb[0:64, 0:64]), ["og", "identb"], [PSK(0)], inc=(half == 1 and k == 3))
            P.op("act", I("activation", out=mixT[:, :, :], in_=pT.rearrange("p (k n) -> p k n", k=8), func=AF.Copy), [PSK(0)], ["mixT"])
            P.dma("sp", I("dma_start", out=h2[:, :], in_=x[t * 128:(t + 1) * 128, :]), [], [("h2_%d" % tp)])
            for nb in range(2):
                for k in range(8):
                    P.op("pe", I("matmul", psb[1 + nb][:, :], lhsT=mixT[:, k, :], rhs=wout_bf[:, k, nb * 512:(nb + 1) * 512], start=(k == 0), stop=(k == 7)),
                         ["mixT", "wout_bf"], [PSK(1 + nb)], inc=(k == 7))
                P.op("dve", I("tensor_tensor", out=h2[:, nb * 512:(nb + 1) * 512], in0=h2[:, nb * 512:(nb + 1) * 512], in1=psb[1 + nb][:, :], op=ALU.add),
                     [("h2_%d" % tp), PSK(1 + nb)], [("h2_%d" % tp)])
            r = rms(h2[:, :], 128, D, "statE", 0, ("h2_%d" % tp))
            P.op("dve", I("scalar_tensor_tensor", out=hn[:, :], in0=h2[:, :], scalar=r, in1=ln2b[:, :], op0=ALU.mult, op1=ALU.mult), [("h2_%d" % tp), "statE", "ln2b"], [("hn_%d" % tp)])
            P.op("act", I("activation", out=hs_bf[:, :], in_=h2[:, :], func=AF.Copy, scale=r), [("h2_%d" % tp), "statE"], ["hs_bf"])
            pT3 = psb[3][:].bitcast(BF16)
            for k in range(8):
                P.op("pe", I("transpose", out=pT3[:, k * 128:(k + 1) * 128], in_=hs_bf[:, k * 128:(k + 1) * 128], identity=identb[:, :]),
                     ["hs_bf", "identb"], [PSK(3)], inc=(k == 7))
            P.op("act", I("activation", out=hT[:, :, :], in_=pT3.rearrange("p (k n) -> p k n", k=8), func=AF.Copy), [PSK(3)], ["hT"])
            for g4 in range(4):
                bank = 4 + (g4 % 2)
                ws = wpiece[0] % 2
                wpiece[0] += 1
                P.dma("sp", I("dma_start", out=wqs[ws][:, :, :], in_=wq_dram[:, g4 * 512:(g4 + 1) * 512].rearrange("(k p) c -> p k c", p=128)), ["wq_dram"], ["wqs%d" % ws])
                for j in range(4):
                    hp = g4 * 4 + j
                    for k in range(8):
                        P.op("pe", I("matmul", psb[bank][:, j * 128:(j + 1) * 128], lhsT=wqs[ws][:, k, j * 128:(j + 1) * 128], rhs=hT[:, k, :], start=(k == 0), stop=(k == 7)),
                             ["wqs%d" % ws, "hT"], [PSK(bank)], inc=(k == 7))
                if g4 % 2 == 0:
                    P.op("act", I("activation", out=qT_sb[:, g4 * 4:(g4 + 1) * 4, :], in_=psb[bank][:, :].rearrange("p (j n) -> p j n", j=4), func=AF.Copy), [PSK(bank)], ["qT_sb"])
                else:
                    P.op("dve", I("tensor_copy", out=qT_sb[:, g4 * 4:(g4 + 1) * 4, :], in_=psb[bank][:, :].rearrange("p (j n) -> p j n", j=4)), [PSK(bank)], ["qT_sb"])
            for g4 in range(4):
                bank = g4
                for j in range(4):
                    hp = g4 * 4 + j
                    P.op("pe", I("matmul", psb[bank][:, j * 128:(j + 1) * 128], lhsT=qT_sb[:, hp, :], rhs=keys_sb[:, hp * 128:(hp + 1) * 128], start=True, stop=True),
                         ["qT_sb", "keys_sb"], [PSK(bank)], inc=(j == 3))
                if g4 % 2 == 0:
                    P.op("act", I("activation", out=s_sb[:, g4 * 4:(g4 + 1) * 4, :], in_=psb[bank][:, :].rearrange("p (j n) -> p j n", j=4), func=AF.Copy), [PSK(bank)], ["s_sb"])
                else:
                    P.op("dve", I("tensor_copy", out=s_sb[:, g4 * 4:(g4 + 1) * 4, :], in_=psb[bank][:, :].rearrange("p (j n) -> p j n", j=4)), [PSK(bank)], ["s_sb"])
            L1a.append(P.rec)
            P.rec = []
            for hp in range(16):
                P.op("dve", I("max", out=ts[:, hp, 0:8], in_=s_sb[:, hp, :]), ["s_sb"], ["ts"])
                P.op("dve", I("max_index", out=ti[:, hp, 0:8], in_max=ts[:, hp, 0:8], in_values=s_sb[:, hp, :]), ["s_sb", "ts"], ["ti"])
                P.op("dve", I("match_replace", out=s_sb[:, hp, :], in_to_replace=ts[:, hp, 0:8], in_values=s_sb[:, hp, :], imm_value=NEG), ["s_sb", "ts"], ["s_sb"])
                P.op("dve", I("max", out=ts[:, hp, 8:16], in_=s_sb[:, hp, :]), ["s_sb"], ["ts"])
                P.op("dve", I("max_index", out=ti[:, hp, 8:16], in_max=ts[:, hp, 8:16], in_values=s_sb[:, hp, :]), ["s_sb", "ts"], ["ti"])
            P.op("dve", I("tensor_copy", out=tif[:, :, :], in_=ti[:, :, :]), ["ti"], ["tif"])
            for h in range(8):
                cb = h % 2
                ck = "cand%d" % cb
                c3 = cand[:, cb, :].rearrange("p (a b) -> p a b", a=16)
                P.op("dve", I("tensor_tensor", out=c3, in0=ts[:, 2 * h, :].unsqueeze(2).broadcast_to([128, 16, 16]),
                              in1=ts[:, 2 * h + 1, :].unsqueeze(1).broadcast_to([128, 16, 16]), op=ALU.add), ["ts"], [ck])
                P.op("dve", I("max", out=bs[:, h, 0:8], in_=cand[:, cb, :]), [ck], ["bs"])
                P.op("dve", I("max_index", out=pos[:, h, 0:8], in_max=bs[:, h, 0:8], in_values=cand[:, cb, :]), [ck, "bs"], ["pos"])
                P.op("dve", I("match_replace", out=cand[:, cb, :], in_to_replace=bs[:, h, 0:8], in_values=cand[:, cb, :], imm_value=NEG), [ck, "bs"], [ck])
                P.op("dve", I("max", out=bs[:, h, 8:16], in_=cand[:, cb, :]), [ck], ["bs"])
                P.op("dve", I("max_index", out=pos[:, h, 8:16], in_max=bs[:, h, 8:16], in_values=cand[:, cb, :]), [ck, "bs"], ["pos"])
            pos2 = pos[:, :, :].rearrange("p h k -> p (h k)")
            P.op("dve", I("tensor_single_scalar", out=ai[:, :], in_=pos2, scalar=4, op=ALU.logical_shift_right), ["pos"], ["ai"])
            P.op("dve", I("tensor_copy", out=af[:, 0, :], in_=ai[:, :]), ["ai"], ["af"])
            P.op("dve", I("tensor_single_scalar", out=ai[:, :], in_=pos2, scalar=15, op=ALU.bitwise_and), ["pos", "af"], ["ai"])
            P.op("dve", I("tensor_copy", out=af[:, 1, :], in_=ai[:, :]), ["ai"], ["af"])
            for li in range(2):
                for half in range(2):
                    oh = prod[half].rearrange("p (a b) -> p a b", b=16)
                    oh4 = prod[half].rearrange("p (h k b) -> p h k b", h=4, k=16)
                    okey = "s_sb"
                    P.op("dve", I("tensor_tensor", out=oh, in0=af[:, li, half * 64:(half + 1) * 64].unsqueeze(2).broadcast_to([128, 64, 16]),
                                  in1=iota16.unsqueeze(1).broadcast_to([128, 64, 16]), op=ALU.is_equal), ["af", "iota"], [okey])
                    tl = tif[:, half * 8:(half + 1) * 8, :].rearrange("p (h two) a -> p h two a", two=2)[:, :, li, :]
                    P.op("dve", I("tensor_tensor", out=oh4, in0=oh4, in1=tl.unsqueeze(2).broadcast_to([128, 4, 16, 16]), op=ALU.mult), [okey, "tif"], [okey])
                    P.op("dve", I("tensor_reduce", out=rsel[:, li, half * 64:(half + 1) * 64], in_=oh, axis=AX.X, op=ALU.add), [okey], ["rsel"])
            P.op("dve", I("scalar_tensor_tensor", out=idxf[:, :], in0=rsel[:, 0, :], scalar=128.0, in1=rsel[:, 1, :], op0=ALU.mult, op1=ALU.add), ["rsel"], ["idxf"])
            P.op("dve", I("tensor_copy", out=idxi[:, :], in_=idxf[:, :]), ["idxf"], [("idxi_%d" % tp)])
            g3 = gate[:, :].rearrange("p (h k) -> p h k", h=8)
            P.op("dve", I("tensor_tensor", out=g3, in0=bs[:, :, :], in1=bs[:, :, 0:1].broadcast_to([128, 8, 16]), op=ALU.subtract), ["bs"], [("gate_%d" % tp)])
            P.op("act", I("activation", out=gate[:, :], in_=gate[:, :], func=AF.Exp), [("gate_%d" % tp)], [("gate_%d" % tp)])
            P.op("dve", I("tensor_reduce", out=gz[:, 0:8], in_=g3, axis=AX.X, op=ALU.add), [("gate_%d" % tp)], ["gz"])
            P.op("dve", I("reciprocal", out=gz[:, 8:16], in_=gz[:, 0:8]), ["gz"], ["gz"])
            P.op("dve", I("tensor_tensor", out=g3, in0=g3, in1=gz[:, 8:16].unsqueeze(2).broadcast_to([128, 8, 16]), op=ALU.mult), [("gate_%d" % tp), "gz"], [("gate_%d" % tp)])
            L1.append(P.rec)
            P.rec = []
            LAG = 4
            gi0 = gi
            for step in range(128 + LAG):
                if step < 128:
                    hk = step
                    b_ = (gi0 + hk) % NG
                    P.dma("pool", I("indirect_dma_start", out=gbuf[b_][:, :], out_offset=None, in_=uv_bf[:, :],
                                    in_offset=bass.IndirectOffsetOnAxis(ap=idxi[:, hk:hk + 1], axis=0)), [("idxi_%d" % tp)], ["gbuf%d" % b_])
                    pb = hk % 3
                    P.op("dve", I("tensor_tensor", out=prodb[pb][:, :], in0=gbuf[b_][:, 0:D], in1=hn[:, :], op=ALU.mult), ["gbuf%d" % b_, ("hn_%d" % tp)], ["prodb%d" % pb])
                    P.op("act", I("activation", out=junkb[:, :], in_=prodb[pb][:, :], func=AF.Copy, accum_out=apre[:, hk:hk + 1]), ["prodb%d" % pb], ["junkb", "apre%d" % hk])
                    P.op("act", I("activation", out=gel[:, hk:hk + 1], in_=apre[:, hk:hk + 1], func=AF.Gelu), ["apre%d" % hk], ["gel%d" % hk])
                if step >= LAG:
                    hk = step - LAG
                    b_ = (gi0 + hk) % NG
                    dg = hk % 4
                    P.op("dve", I("tensor_scalar", out=diag[dg][:, :], in0=identf[:, :], scalar1=gel[:, hk:hk + 1], scalar2=gate[:, hk:hk + 1], op0=ALU.mult, op1=ALU.mult),
                         ["identf", "gel%d" % hk, ("gate_%d" % tp)], ["diag%d" % dg])
                    for nb in range(2):
                        P.op("pe", I("matmul", psb[6 + nb][:, :], lhsT=diag[dg][:, :], rhs=gbuf[b_][:, D + nb * 512:D + (nb + 1) * 512], start=(hk == 0), stop=(hk == 127)),
                             ["diag%d" % dg, "gbuf%d" % b_], [PSK(6 + nb)], inc=(nb == 1))
            gi = gi0 + 128
            for nb in range(2):
                P.op("dve", I("tensor_tensor", out=h2[:, nb * 512:(nb + 1) * 512], in0=h2[:, nb * 512:(nb + 1) * 512], in1=psb[6 + nb][:, :], op=ALU.add),
                     [("h2_%d" % tp), PSK(6 + nb)], [("h2_%d" % tp)])
            r = rms(h2[:, :], 128, D, "statF", 4, ("h2_%d" % tp))
            P.op("dve", I("scalar_tensor_tensor", out=outsb, in0=h2[:, :], scalar=r, in1=finb[:, :], op0=ALU.mult, op1=ALU.mult), [("h2_%d" % tp), "statF", "finb"], ["qT_sb"])
            P.dma("sp", I("dma_start", out=out[t * 128:(t + 1) * 128, :], in_=outsb), ["qT_sb"], [], final=True)
            L2.append(P.rec)
            P.rec = None

        def replay_group(lst, i):
            P.replay(lst[i])
            i += 1
            while i < len(lst) and lst[i - 1][0] == "op" and lst[i - 1][1] == "pe" and lst[i - 1][5] is False:
                P.replay(lst[i])
                i += 1
            return i

        def replay_all(lst):
            i = 0
            while i < len(lst):
                i = replay_group(lst, i)

        replay_all(L1a[0])
        replay_all(L1[0])
        for t in range(16):
            if t + 1 < 16:
                replay_all(L1a[t + 1])
            cur = L2[t]
            nxt = L1[t + 1] if t + 1 < 16 else []
            ci = 0
            ni = 0
            cnt = 0
            while ci < len(cur):
                ci = replay_group(cur, ci)
                cnt += 1
                if INTERLEAVE and cnt % INTERLEAVE == 0 and ni < len(nxt):
                    ni = replay_group(nxt, ni)
            while ni < len(nxt):
                ni = replay_group(nxt, ni)
        P.emit()
    return nc


def _consts():
    c = {}
    c["c_identb"] = np.eye(128, dtype=np.float32).astype(ml_dtypes.bfloat16)
    c["c_identf"] = np.eye(128, dtype=np.float32)
    tri = np.zeros((64, 256), np.float32)
    j = np.arange(64)[:, None]
    i = np.arange(64)[None, :]
    tri[:, 0:64] = (j <= i)
    tri[:, 64:128] = (j >= i)
    tri[:, 128:192] = 1.0
    tri[:, 192] = 1.0
    c["c_tri"] = tri
    inv = np.power(np.float32(500000.0), -np.arange(0, 16, 2, dtype=np.float32) / np.float32(16)).astype(np.float32)
    cs = np.zeros((128, 17 * 16), np.float32)
    for t in range(17):
        if t < 16:
            pos = 16 + t * 128 + np.arange(128)
        else:
            pos = np.concatenate([np.arange(16), np.zeros(112)])
        ang = pos.astype(np.float32)[:, None] * inv[None, :]
        cs[:, t * 16:t * 16 + 8] = np.cos(ang)
        cs[:, t * 16 + 8:t * 16 + 16] = np.sin(ang)
    c["c_cs"] = cs
    c["c_iota"] = np.broadcast_to(np.arange(256, dtype=np.float32)[None, :], (128, 256)).copy()
    return c


def _prep(inp):
    f = lambda a: np.ascontiguousarray(np.asarray(a, dtype=np.float32))
    bro = lambda v, n: np.ascontiguousarray(np.broadcast_to(f(v).reshape(1, -1), (128, n)))
    m = {}
    m["meta"] = f(inp["meta_tokens"])
    m["w_in"] = f(inp["w_in"][0])
    m["ln1c"] = f(np.asarray(inp["ln1_w"][0]).reshape(8, 128).T)
    m["w_out"] = f(inp["w_out"][0])
    m["ln2c"] = f(np.asarray(inp["ln2_w"][0]).reshape(8, 128).T)
    m["ln2_b"] = bro(inp["ln2_w"][0], 1024)
    m["fin_b"] = bro(inp["final_norm_w"], 1024)
    m["w_q"] = f(inp["peer_w_q"][0])
    sk = np.asarray(inp["peer_sub_keys"][0], dtype=np.float32)
    m["keysT"] = f(sk.transpose(3, 0, 1, 2).reshape(128, 2048))
    m["peer_uv"] = np.ascontiguousarray(np.concatenate([np.asarray(inp["peer_u"][0], dtype=np.float32), np.asarray(inp["peer_v"][0], dtype=np.float32)], axis=1))
    m["subln_b"] = bro(inp["da_subln_w"][0], 128)
    m["glan_b"] = bro(inp["gla_norm_w"][0], 128)
    lam = np.concatenate([np.asarray(inp[k][0], dtype=np.float32) for k in ("da_lambda_q1", "da_lambda_k1", "da_lambda_q2", "da_lambda_k2")])
    m["lamv"] = bro(lam, 256)
    w2 = np.zeros((32, 512), np.float32)
    w2[0:16, 0:256] = np.asarray(inp["gla_gate_w2_f"][0])
    w2[16:32, 256:512] = np.asarray(inp["gla_gate_w2_b"][0])
    m["w2cat"] = w2
    m["bcat"] = f(np.concatenate([np.asarray(inp["gla_gate_b_f"][0]), np.asarray(inp["gla_gate_b_b"][0])]).reshape(1, 512))
    m.update(_consts())
    return m


_NC_CACHE = {}


def kernel(**inputs):
    shared = _prep(inputs)
    xs_ = np.asarray(inputs["x"], dtype=np.float32)
    if "nc" not in _NC_CACHE:
        _NC_CACHE["nc"] = build()
    nc = _NC_CACHE["nc"]
    in_maps = []
    for b in range(8):
        m = dict(shared)
        m["x"] = np.ascontiguousarray(xs_[b])
        in_maps.append(m)
    res = run_bass_kernel_spmd(nc, in_maps, core_ids=list(range(8)))
    return np.stack([np.asarray(r["out"], dtype=np.float32) for r in res.results], axis=0)
```

```python
import numpy as np
import concourse.bass as bass
import concourse.mybir as mybir
from concourse.bass_utils import run_bass_kernel_spmd

F32 = mybir.dt.float32
BF16 = mybir.dt.bfloat16
I32 = mybir.dt.int32
U32 = mybir.dt.uint32
ALU = mybir.AluOpType
AF = mybir.ActivationFunctionType
AX = mybir.AxisListType

ENGS = ("pe", "act", "dve", "pool", "sp")
NSLOT = 12


class Prog:
    def __init__(self, nc, es):
        self.nc = nc
        self.q = {e: [] for e in ENGS}
        self.cnt = {e: 0 for e in ENGS}
        self.waited = {e: {} for e in ENGS}
        self.lastw = {}
        self.readers = {}
        self.pending = {e: ([], []) for e in ENGS}
        self.sems = {}
        for e in ENGS:
            self.sems[e] = es.enter_context(nc.semaphore("s_" + e))
        self.dslot = {}
        self.duse = {}
        self.dnext = {}
        for e in ("sp", "act", "pool"):
            self.dnext[e] = 0
            for i in range(NSLOT):
                k = ("d", e, i)
                self.sems[k] = es.enter_context(nc.semaphore("d_%s_%d" % (e, i)))
                self.duse[k] = 0
        self.final = []
        self.rec = None

    def _need(self, eng, dep, waits):
        if dep is None:
            return
        k, v = dep
        if k == "pe" and eng == "pe":
            return
        if self.waited[eng].get(k, 0) >= v:
            return
        self.waited[eng][k] = v
        waits.append((k, v))

    def _deps(self, eng, reads, writes):
        waits = []
        for r in reads:
            self._need(eng, self.lastw.get(r), waits)
        for w in writes:
            self._need(eng, self.lastw.get(w), waits)
            for d in self.readers.get(w, ()):
                self._need(eng, d, waits)
        return waits

    def _commit(self, token, reads, writes):
        for r in reads:
            self.readers.setdefault(r, []).append(token)
        for w in writes:
            self.lastw[w] = token
            self.readers[w] = []

    def op(self, eng, fn, reads=(), writes=(), inc=True):
        if self.rec is not None:
            self.rec.append(("op", eng, fn, list(reads), list(writes), inc))
            return
        waits = self._deps(eng, reads, writes)
        pr, pw = self.pending[eng]
        pr.extend(reads)
        pw.extend(writes)
        if inc:
            self.cnt[eng] += 1
            token = (eng, self.cnt[eng])
            self._commit(token, pr, pw)
            self.pending[eng] = ([], [])
            self.q[eng].append((waits, fn, eng, 1))
        else:
            self.q[eng].append((waits, fn, None, 0))

    def dma(self, eng, fn, reads=(), writes=(), final=False):
        if self.rec is not None:
            self.rec.append(("dma", eng, fn, list(reads), list(writes), final))
            return
        waits = self._deps(eng, reads, writes)
        i = self.dnext[eng]
        self.dnext[eng] = (i + 1) % NSLOT
        k = ("d", eng, i)
        if self.duse[k] > 0:
            self._need(eng, (k, 16 * self.duse[k]), waits)
        self.duse[k] += 1
        token = (k, 16 * self.duse[k])
        self._commit(token, reads, writes)
        self.q[eng].append((waits, fn, k, 16))
        if final:
            self.final.append(token)

    def replay(self, r):
        if r[0] == "op":
            self.op(r[1], r[2], r[3], r[4], r[5])
        else:
            self.dma(r[1], r[2], r[3], r[4], r[5])

    def barrier(self):
        toks = [(e, self.cnt[e]) for e in ENGS if self.cnt[e] > 0]
        for k, n in self.duse.items():
            if n > 0:
                toks.append((k, 16 * n))
        for e in ENGS:
            waits = []
            for tok in toks:
                self._need(e, tok, waits)
            if waits:
                self.q[e].append((waits, None, None, 0))

    def emit(self):
        nc = self.nc
        fw = []
        for tok in self.final:
            self._need("sp", tok, fw)
        self.q["sp"].append((fw, None, None, 0))
        with nc.Block() as block:
            def run(engname):
                def body(e):
                    for waits, fn, sk, iv in self.q[engname]:
                        for k, v in waits:
                            e.wait_ge(self.sems[k], v)
                        if fn is None:
                            continue
                        if isinstance(fn, tuple):
                            ins = getattr(e, fn[0])(*fn[1], **fn[2])
                        else:
                            ins = fn(e)
                        if sk is not None:
                            ins.then_inc(self.sems[sk], iv)
                return body

            block.sync(run("sp"))
            block.scalar(run("act"))
            block.vector(run("dve"))
            block.gpsimd(run("pool"))
            block.tensor(run("pe"))
from contextlib import ExitStack
import ml_dtypes

D = 1024
T_REAL = 2048
NMETA = 16
EPS = 1e-6
IN_W = 3104
NEG = -1.0e30


def I(name, *a, **k):
    return (name, a, k)


def rgroup(P, lst, i):
    P.replay(lst[i])
    i += 1
    while i < len(lst) and lst[i - 1][0] == "op" and lst[i - 1][1] == "pe" and lst[i - 1][5] is False:
        P.replay(lst[i])
        i += 1
    return i


def build(dbg=None):
    nc = bass.Bass("TRN2", target_bir_lowering=False)

    def din(name, shape, dt=F32):
        return nc.dram_tensor(name, list(shape), dt, kind="ExternalInput").ap()

    x = din("x", [2048, D])
    meta = din("meta", [16, D])
    w_in = din("w_in", [D, IN_W])
    ln1c = din("ln1c", [128, 8])
    w_out = din("w_out", [D, D])
    ln2c = din("ln2c", [128, 8])
    ln2_b = din("ln2_b", [128, D])
    fin_b = din("fin_b", [128, D])
    w_q = din("w_q", [D, 2048])
    keysT = din("keysT", [128, 2048])
    peer_uv = din("peer_uv", [16384, 2 * D])
    uv_bf = nc.dram_tensor("uv_bf", [16384, 2 * D], BF16, kind="Internal").ap()
    wq_dram = nc.dram_tensor("wq_dram", [D, 2048], BF16, kind="Internal").ap()
    wo_dram = nc.dram_tensor("wo_dram", [D, D], BF16, kind="Internal").ap()
    subln_b = din("subln_b", [128, 128])
    glan_b = din("glan_b", [128, 128])
    lamv = din("lamv", [128, 256])
    w2cat = din("w2cat", [32, 512])
    bcat = din("bcat", [1, 512])
    c_identb = din("c_identb", [128, 128], BF16)
    c_identf = din("c_identf", [128, 128])
    c_tri = din("c_tri", [64, 256])
    c_cs = din("c_cs", [128, 17 * 16])
    c_iota = din("c_iota", [128, 256])
    out = nc.dram_tensor("out", [2048, D], F32, kind="ExternalOutput").ap()
    dbg_out = None
    if dbg is not None:
        dbg_out = nc.dram_tensor("dbg", list(dbg[1]), F32, kind="ExternalOutput").ap()

    es = ExitStack()
    with es:
        P = Prog(nc, es)

        def sb(name, shape, dt=F32, stack=es):
            return stack.enter_context(nc.sbuf_tensor(name, list(shape), dt))

        psb = [es.enter_context(nc.psum_tensor("ps%d" % i, [128, 512], F32)) for i in range(8)]

        def PSK(i):
            return "ps%d" % i

        identb = sb("identb", [128, 128], BF16)
        identf = sb("identf", [128, 128])
        iota = sb("iota", [128, 256])
        ln2 = sb("ln2", [128, 8])
        mix_da = sb("mix_da", [128, 16, 512], BF16)
        og_full = sb("og", [128, 32, 512], BF16)
        og = og_full[0:64]
        stat = sb("stat", [128, 16])
        junkb = sb("junkb", [128, 1024], BF16)
        junkd = junkb
        stK = ExitStack()
        es.enter_context(stK)
        tri = sb("tri", [64, 256], F32, stK)
        cs = sb("cs", [128, 17 * 16], F32, stK)
        sublnb = sb("sublnb", [128, 128], F32, stK)
        glanb = sb("glanb", [128, 128], F32, stK)
        lam_t = sb("lam_t", [128, 256], F32, stK)
        lam_s = sb("lam_s", [128, 8], F32, stK)
        w2c = sb("w2c", [32, 512], F32, stK)
        bc = sb("bc", [1, 512], F32, stK)
        ones_row = sb("ones_row", [1, 128], F32, stK)
        ln1 = sb("ln1", [128, 8], F32, stK)

        def ld(eng, dst, src, key):
            P.dma(eng, I("dma_start", out=dst, in_=src), [], [key])

        ld("sp", identb[:], c_identb, "identb")
        ld("sp", identf[:], c_identf, "identf")
        ld("sp", tri[:], c_tri, "tri")
        ld("sp", cs[:], c_cs, "cs")
        ld("sp", iota[:], c_iota, "iota")
        ld("act", sublnb[:], subln_b, "sublnb")
        ld("act", glanb[:], glan_b, "glanb")
        ld("act", lam_t[:], lamv, "lam_t")
        ld("act", w2c[:], w2cat, "w2c")
        ld("act", bc[:], bcat, "bc")
        ld("act", ln1[:], ln1c, "ln1")
        ld("act", ln2[:], ln2c, "ln2")
        P.op("dve", I("memset", ones_row[:], 1.0), [], ["ones_row"])
        triF = tri[:, 0:64]
        triB = tri[:, 64:128]
        ones64 = tri[:, 128:192]
        onescol = tri[:, 192:193]

        P.op("dve", I("scalar_tensor_tensor", out=junkb[:, 0:64], in0=lam_t[:, 0:64], scalar=1.0, in1=lam_t[:, 64:128],
                                                    op0=ALU.mult, op1=ALU.mult, accum_out=lam_s[:, 0:1]), ["lam_t"], ["junkb", "lam_s"])
        P.op("dve", I("scalar_tensor_tensor", out=junkb[:, 0:64], in0=lam_t[:, 128:192], scalar=1.0, in1=lam_t[:, 192:256],
                                                    op0=ALU.mult, op1=ALU.mult, accum_out=lam_s[:, 1:2]), ["lam_t", "junkb"], ["junkb", "lam_s"])
        P.op("act", I("activation", out=lam_s[:, 2:4], in_=lam_s[:, 0:2], func=AF.Exp), ["lam_s"], ["lam_s"])
        P.op("dve", I("scalar_tensor_tensor", out=lam_s[:, 4:5], in0=lam_s[:, 3:4], scalar=-0.2, in1=lam_s[:, 2:3],
                                                    op0=ALU.add, op1=ALU.subtract), ["lam_s"], ["lam_s"])
        nlam = lam_s[:, 4:5]
        P.op("dve", I("tensor_scalar", out=sublnb[:], in0=sublnb[:], scalar1=0.8, scalar2=None, op0=ALU.mult), ["sublnb"], ["sublnb"])

        def rms(src_ap, Pn, width, skey, col, srckey):
            P.op("dve", I("scalar_tensor_tensor", out=junkd[0:Pn, 0:width], in0=src_ap, scalar=1.0, in1=src_ap, op0=ALU.mult, op1=ALU.mult,
                          accum_out=stat[0:Pn, col:col + 1]), [srckey], ["junkd", skey])
            P.op("act", I("activation", out=stat[0:Pn, col + 1:col + 2], in_=stat[0:Pn, col:col + 1], func=AF.Ln, bias=EPS, scale=1.0 / width), [skey], [skey])
            P.op("act", I("activation", out=stat[0:Pn, col + 2:col + 3], in_=stat[0:Pn, col + 1:col + 2], func=AF.Exp, scale=-0.5), [skey], [skey])
            return stat[0:Pn, col + 2:col + 3]

        st1 = ExitStack()
        stK.enter_context(st1)
        w_bf = sb("w_bf", [128, 8, IN_W], BF16, st1)
        st1b = ExitStack()
        st1.enter_context(st1b)
        qkT = sb("qkT", [128, 12, 2064], BF16, st1b)
        vtok = sb("vtok", [128, 17, 4, 129], BF16, st1b)
        stA = ExitStack()
        st1b.enter_context(stA)
        wst = [sb("wst%d" % i, [128, IN_W], F32, stA) for i in range(2)]
        for c in range(8):
            s = c % 2
            P.dma("sp" if s == 0 else "act", I("dma_start", out=wst[s][:], in_=w_in[c * 128:(c + 1) * 128, :]), [], ["wst%d" % s])
            if s == 0:
                P.op("dve", I("tensor_scalar", out=w_bf[:, c, :], in0=wst[s][:], scalar1=ln1[:, c:c + 1], scalar2=None, op0=ALU.mult),
                     ["wst%d" % s, "ln1"], ["w_bf"])
            else:
                P.op("act", I("activation", out=w_bf[:, c, :], in_=wst[s][:], func=AF.Copy, scale=ln1[:, c:c + 1]), ["wst%d" % s, "ln1"], ["w_bf"])
        P.op("dve", I("memset", vtok[:, :, :, 128:129], 1.0), [], ["vtok"])
        P.op("dve", I("memset", qkT[64:128, 0:4, :], 0.0), [], ["qkT"])
        P.op("dve", I("memset", qkT[0:64, 8:12, :], 0.0), [], ["qkT"])
        stA.close()
        P.barrier()

        stB = ExitStack()
        st1b.enter_context(stB)
        xt = [sb("xt%d" % i, [128, D], F32, stB) for i in range(2)]
        xs = [sb("xs%d" % i, [128, D], BF16, stB) for i in range(2)]
        xT = [sb("xT%d" % i, [128, 8, 128], BF16, stB) for i in range(2)]
        qk_sb = [sb("qk_sb%d" % i, [128, 1024], F32, stB) for i in range(2)]
        qk_bf = [sb("qk_bf%d" % i, [128, 1024], BF16, stB) for i in range(2)]
        rp = [sb("rp%d" % i, [128, 4, 16, 8], F32, stB) for i in range(2)]

        def load_norm_T(t_rows_ap, Pn, s, statcol, psbank):
            P.dma("sp", I("dma_start", out=xt[s][0:Pn, :], in_=t_rows_ap), [], ["xt%d" % s])
            r = rms(xt[s][0:Pn, :], Pn, D, "stat%d" % statcol, statcol, "xt%d" % s)
            P.op("dve", I("tensor_scalar", out=xs[s][0:Pn, :], in0=xt[s][0:Pn, :], scalar1=r, scalar2=None, op0=ALU.mult),
                 ["xt%d" % s, "stat%d" % statcol], ["xs%d" % s])
            pT = psb[psbank][:].bitcast(BF16)
            for k in range(8):
                P.op("pe", I("transpose", out=pT[:, k * 128:k * 128 + Pn], in_=xs[s][0:Pn, k * 128:(k + 1) * 128], identity=identb[0:Pn, 0:Pn]),
                     ["xs%d" % s, "identb"], [PSK(psbank)], inc=(k == 7))
            P.op("act", I("activation", out=xT[s][:, :, 0:Pn], in_=pT.rearrange("p (k n) -> p k n", k=8)[:, :, 0:Pn], func=AF.Copy),
                 [PSK(psbank)], ["xT%d" % s])

        A_lists = []
        for t in range(17):
            if dbg is None:
                P.rec = []
            s = t % 2
            Pn = 128 if t < 16 else 16
            rows = x[t * 128:(t + 1) * 128, :] if t < 16 else meta
            tok0 = t * 128
            load_norm_T(rows, Pn, s, (t % 2) * 4, 4 + (t % 2))
            for g in range(3):
                for k in range(8):
                    P.op("pe", I("matmul", psb[g][0:Pn, :], lhsT=xT[s][:, k, 0:Pn], rhs=w_bf[:, k, g * 512:(g + 1) * 512],
                                                           start=(k == 0), stop=(k == 7)),
                         ["xT%d" % s, "w_bf"], [PSK(g)], inc=(k == 7))
            P.op("act", I("activation", out=qk_sb[s][0:Pn, 0:512], in_=psb[0][0:Pn, :], func=AF.Copy, scale=0.125), [PSK(0)], ["qk_sb%d" % s])
            P.op("act", I("activation", out=qk_sb[s][0:Pn, 512:1024], in_=psb[1][0:Pn, :], func=AF.Copy), [PSK(1)], ["qk_sb%d" % s])
            P.op("dve", I("tensor_copy", out=vtok[0:Pn, t, :, 0:128], in_=psb[2][0:Pn, :].rearrange("p (h d) -> p h d", h=4)), [PSK(2)], ["vtok"])

            if dbg is not None and dbg[0] == "A1" and t == dbg[2]:
                dt_ = sb("dbgt", [128, 4096], F32)
                P.op("dve", I("memset", dt_[:], 0.0), [], ["dbgt"])
                P.op("dve", I("tensor_copy", out=dt_[0:Pn, 0:1024], in_=xs[s][0:Pn, :]), ["xs%d" % s], ["dbgt"])
                P.op("dve", I("tensor_copy", out=dt_[:, 1024:2048], in_=w_bf[:, 1, 0:1024]), ["w_bf"], ["dbgt"])
                P.op("dve", I("tensor_copy", out=dt_[:, 2048:3072].rearrange("p (k n) -> p k n", k=8), in_=xT[s][:, :, :]), ["xT%d" % s], ["dbgt"])
                P.op("dve", I("tensor_copy", out=dt_[0:Pn, 3072:4096], in_=qk_sb[s][0:Pn, :]), ["qk_sb%d" % s], ["dbgt"])
                P.dma("sp", I("dma_start", out=dbg_out, in_=dt_[:]), ["dbgt"], [], final=True)
                P.emit()
                return nc
            qv = qk_sb[s][0:Pn, :].rearrange("p (g d) -> p g d", g=16)
            x1 = qv[:, :, 0:8]
            x2 = qv[:, :, 8:16]
            cosb = cs[0:Pn, t * 16:t * 16 + 8].unsqueeze(1).broadcast_to([Pn, 16, 8])
            sinb = cs[0:Pn, t * 16 + 8:t * 16 + 16].unsqueeze(1).broadcast_to([Pn, 16, 8])
            rk = "rp%d" % s
            P.op("pool", I("tensor_tensor", out=rp[s][0:Pn, 0], in0=x1, in1=cosb, op=ALU.mult), ["qk_sb%d" % s, "cs"], [rk])
            P.op("pool", I("tensor_tensor", out=rp[s][0:Pn, 1], in0=x2, in1=sinb, op=ALU.mult), ["qk_sb%d" % s, "cs"], [rk])
            P.op("pool", I("tensor_tensor", out=rp[s][0:Pn, 2], in0=x2, in1=cosb, op=ALU.mult), ["qk_sb%d" % s, "cs"], [rk])
            P.op("pool", I("tensor_tensor", out=rp[s][0:Pn, 3], in0=x1, in1=sinb, op=ALU.mult), ["qk_sb%d" % s, "cs"], [rk])
            P.op("dve", I("tensor_copy", out=qk_bf[s][0:Pn, :], in_=qk_sb[s][0:Pn, :]), ["qk_sb%d" % s], ["qk_bf%d" % s])
            qbv = qk_bf[s][0:Pn, :].rearrange("p (g d) -> p g d", g=16)
            P.op("dve", I("tensor_tensor", out=qbv[:, :, 0:8], in0=rp[s][0:Pn, 0], in1=rp[s][0:Pn, 1], op=ALU.subtract), [rk], ["qk_bf%d" % s])
            P.op("dve", I("tensor_tensor", out=qbv[:, :, 8:16], in0=rp[s][0:Pn, 2], in1=rp[s][0:Pn, 3], op=ALU.add), [rk], ["qk_bf%d" % s])
            bank = 6 + (t % 2)
            pT = psb[bank][:].bitcast(BF16)
            for j in range(8):
                P.op("pe", I("transpose", out=pT[:, j * 128:j * 128 + Pn], in_=qk_bf[s][0:Pn, j * 128:(j + 1) * 128], identity=identb[0:Pn, 0:Pn]),
                     ["qk_bf%d" % s, "identb"], [PSK(bank)], inc=(j == 7))
            pT3 = pT.rearrange("p (k n) -> p k n", k=8)
            P.op("act", I("activation", out=qkT[0:64, 0:4, tok0:tok0 + Pn], in_=pT3[0:64, 0:4, 0:Pn], func=AF.Copy), [PSK(bank)], ["qkT"])
            P.op("dve", I("tensor_copy", out=qkT[64:128, 8:12, tok0:tok0 + Pn], in_=pT3[64:128, 0:4, 0:Pn]), [PSK(bank)], ["qkT"])
            P.op("act", I("activation", out=qkT[:, 4:8, tok0:tok0 + Pn], in_=pT3[:, 4:8, 0:Pn], func=AF.Copy), [PSK(bank)], ["qkT"])
            if dbg is None:
                A_lists.append(P.rec)
                P.rec = None

            if dbg is not None and dbg[0] == "A2" and t == dbg[2]:
                dt_ = sb("dbgt", [128, 4096], F32)
                P.op("dve", I("memset", dt_[:], 0.0), [], ["dbgt"])
                P.op("dve", I("tensor_copy", out=dt_[0:Pn, 0:1024], in_=qk_bf[s][0:Pn, :]), ["qk_bf%d" % s], ["dbgt"])
                P.op("dve", I("tensor_copy", out=dt_[:, 1024:2048].rearrange("p (k n) -> p k n", k=8)[:, :, 0:Pn], in_=qkT[:, :, tok0:tok0 + Pn]), ["qkT"], ["dbgt"])
                P.op("dve", I("tensor_copy", out=dt_[:, 2048:3072], in_=pT), [PSK(bank)], ["dbgt"])
                for j_ in range(8):
                    P.op("dve", I("tensor_copy", out=dt_[:, 3072 + j_ * 128:3072 + (j_ + 1) * 128], in_=qkT[:, j_, 0:128]), ["qkT"], ["dbgt"])
                P.dma("sp", I("dma_start", out=dbg_out, in_=dt_[:]), ["dbgt"], [], final=True)
                P.emit()
                return nc

        if dbg is None:
            cur = A_lists[0]
            ci = 0
            while ci < len(cur) // 2:
                ci = rgroup(P, cur, ci)
            for n_ in range(len(A_lists)):
                cur = A_lists[n_]
                nxt = A_lists[n_ + 1] if n_ + 1 < len(A_lists) else []
                half_n = len(nxt) // 2
                ni = 0
                while ci < len(cur) or ni < half_n:
                    if ci < len(cur):
                        ci = rgroup(P, cur, ci)
                    if ni < half_n:
                        ni = rgroup(P, nxt, ni)
                ci = ni
        if dbg is not None and dbg[0] == "A":
            dt_ = sb("dbgt", [128, 2064], F32)
            for tt in range(17):
                n_ = 128 if tt < 16 else 16
                P.op("dve", I("tensor_copy", out=dt_[:, tt * 128:tt * 128 + n_], in_=qkT[:, dbg[2], tt * 128:tt * 128 + n_]), ["qkT"], ["dbgt"])
            P.dma("sp", I("dma_start", out=dbg_out, in_=dt_[:]), ["dbgt"], [], final=True)
            P.emit()
            return nc
        stB.close()
        P.barrier()

        stC = ExitStack()
        st1b.enter_context(stC)
        eT_all = sb("eT_all", [128, 17, 512], BF16, stC)
        t0b = [sb("t0b%d" % i, [128, 128], F32, stC) for i in range(2)]
        dab = [sb("dab%d" % i, [128, 128], F32, stC) for i in range(2)]
        rz = [sb("rz%d" % i, [128, 8], F32, stC) for i in range(2)]
        dstat = sb("dstat", [128, 192], F32, stC)
        eTs = [(eT_all, "eT_all"), (og_full[:, 0:17, :], "og")]
        combos = [(qb, h) for qb in range(8) for h in range(4)]
        it_c = [0]
        fin_c = [0]

        def emit_S(i, kts):
            qb, h = combos[i]
            q0 = qb * 256
            eb, ek = eTs[i % 2]
            for kt in kts:
                Pk = 128 if kt < 16 else 16
                k0 = kt * 128
                sbk = it_c[0] % 3
                it_c[0] += 1
                for c in range(2):
                    P.op("pe", I("matmul", psb[sbk][0:Pk, c * 256:(c + 1) * 256], lhsT=qkT[:, 4 + h, k0:k0 + Pk],
                                 rhs=qkT[:, (0 if c == 0 else 8) + h, q0:q0 + 256], start=True, stop=True),
                         ["qkT"], [PSK(sbk)], inc=(c == 1))
                P.op("act", I("activation", out=eb[0:Pk, kt, :], in_=psb[sbk][0:Pk, :], func=AF.Exp), [PSK(sbk)], [ek])

        def emit_PV(i, g):
            qb, h = combos[i]
            eb, ek = eTs[i % 2]
            c, qs = g // 2, g % 2
            bank = 4 + g
            for kt in range(17):
                Pk = 128 if kt < 16 else 16
                P.op("pe", I("matmul", psb[bank][:, 0:129], lhsT=eb[0:Pk, kt, c * 256 + qs * 128:c * 256 + (qs + 1) * 128],
                             rhs=vtok[0:Pk, kt, h, :], start=(kt == 0), stop=(kt == 16)),
                     [ek, "vtok"], [PSK(bank)], inc=(kt == 16))

        def emit_fin(i):
            qb, h = combos[i]
            for qs in range(2):
                f = fin_c[0] % 2
                fin_c[0] += 1
                tile_i = qb * 2 + qs
                ab = 4 + qs
                a0 = psb[4 + qs][:, 0:129]
                a1 = psb[6 + qs][:, 0:129]
                rk = "rz%d" % f
                P.op("dve", I("reciprocal", out=rz[f][:, 0:1], in_=a0[:, 128:129]), [PSK(ab)], [rk])
                P.op("dve", I("reciprocal", out=rz[f][:, 1:2], in_=a1[:, 128:129]), [PSK(ab + 2)], [rk])
                P.op("dve", I("tensor_tensor", out=rz[f][:, 2:3], in0=rz[f][:, 1:2], in1=nlam, op=ALU.mult), [rk, "lam_s"], [rk])
                P.op("dve", I("tensor_scalar", out=t0b[f][:], in0=a0[:, 0:128], scalar1=rz[f][:, 0:1], scalar2=None, op0=ALU.mult),
                     [PSK(ab), rk], ["t0b%d" % f])
                P.op("dve", I("scalar_tensor_tensor", out=dab[f][:], in0=a1[:, 0:128], scalar=rz[f][:, 2:3], in1=t0b[f][:],
                              op0=ALU.mult, op1=ALU.add), [PSK(ab + 2), rk, "t0b%d" % f], ["dab%d" % f])
                sidx = tile_i * 4 + h
                P.op("dve", I("scalar_tensor_tensor", out=junkd[:, 0:128], in0=dab[f][:], scalar=1.0, in1=dab[f][:], op0=ALU.mult, op1=ALU.mult,
                              accum_out=dstat[:, sidx:sidx + 1]), ["dab%d" % f], ["junkd", "dstat"])
                P.op("dve", I("tensor_tensor", out=mix_da[:, tile_i, h * 128:(h + 1) * 128], in0=dab[f][:], in1=sublnb[:], op=ALU.mult),
                     ["dab%d" % f, "sublnb"], ["mix_da"])

        kt_parts = [list(range(0, 5)), list(range(5, 9)), list(range(9, 13)), list(range(13, 17))]
        emit_S(0, list(range(17)))
        for i in range(32):
            for g in range(4):
                emit_PV(i, g)
                if i + 1 < 32:
                    emit_S(i + 1, kt_parts[g])
            emit_fin(i)
        P.op("act", I("activation", out=dstat[:, 64:128], in_=dstat[:, 0:64], func=AF.Ln, bias=EPS, scale=1.0 / 128), ["dstat"], ["dstat"])
        P.op("act", I("activation", out=dstat[:, 128:192], in_=dstat[:, 64:128], func=AF.Exp, scale=-0.5), ["dstat"], ["dstat"])
        m4 = mix_da[:, :, :].rearrange("p t (h d) -> p (t h) d", h=4)
        P.op("dve", I("tensor_tensor", out=m4, in0=m4, in1=dstat[:, 128:192].unsqueeze(2).broadcast_to([128, 64, 128]), op=ALU.mult), ["mix_da", "dstat"], ["mix_da"])
        stC.close()
        if dbg is not None and dbg[0] == "B":
            dt_ = sb("dbgt", [128, 16 * 512], F32)
            P.op("dve", I("tensor_copy", out=dt_[:], in_=mix_da[:].rearrange("p t c -> p (t c)")), ["mix_da"], ["dbgt"])
            P.dma("sp", I("dma_start", out=dbg_out, in_=dt_[:]), ["dbgt"], [], final=True)
            P.emit()
            return nc
        st1b.close()
        P.barrier()

        stG = ExitStack()
        st1.enter_context(stG)
        xc_2 = [sb("xc%d" % i, [64, D], F32, stG) for i in range(2)]
        xcs_2 = [sb("xcs%d" % i, [64, D], BF16, stG) for i in range(2)]
        xTc_2 = [sb("xTc%d" % i, [128, 8, 64], BF16, stG) for i in range(2)]
        gqk_2 = [sb("gqk%d" % i, [64, 512], F32, stG) for i in range(2)]
        gv_bf_2 = [sb("gv_bf%d" % i, [64, 512], BF16, stG) for i in range(2)]
        sgr_2 = [sb("sgr%d" % i, [64, 512], BF16, stG) for i in range(2)]
        z_sb_2 = [sb("z_sb%d" % i, [64, 32], F32, stG) for i in range(2)]
        zT_sb_2 = [sb("zT_sb%d" % i, [32, 64], F32, stG) for i in range(2)]
        e_sb_2 = [sb("e_sb%d" % i, [64, 256], F32, stG) for i in range(2)]
        l_sb_2 = [sb("l_sb%d" % i, [64, 256], F32, stG) for i in range(2)]
        Lc_sb_2 = [sb("Lc_sb%d" % i, [64, 256], F32, stG) for i in range(2)]
        Dm_sb_2 = [sb("Dm_sb%d" % i, [64, 256], F32, stG) for i in range(2)]
        Eq_2 = [sb("Eq%d" % i, [64, 256], F32, stG) for i in range(2)]
        Ek_2 = [sb("Ek%d" % i, [64, 256], F32, stG) for i in range(2)]
        Eh_2 = [sb("Eh%d" % i, [64, 256], F32, stG) for i in range(2)]
        qt_bf_2 = [sb("qt_bf%d" % i, [64, 256], BF16, stG) for i in range(2)]
        kt_bf_2 = [sb("kt_bf%d" % i, [64, 256], BF16, stG) for i in range(2)]
        kh_bf_2 = [sb("kh_bf%d" % i, [64, 256], BF16, stG) for i in range(2)]
        qkTc_2 = [sb("qkTc%d" % i, [64, 8, 64], BF16, stG) for i in range(2)]
        attn_bf_2 = [sb("attn_bf%d" % i, [64, 4, 64], BF16, stG) for i in range(2)]
        S_f = sb("S_f", [64, 4, 128], F32, stG)
        S_bf = sb("S_bf", [64, 4, 128], BF16, stG)
        dec_sb_2 = [sb("dec_sb%d" % i, [64, 4], F32, stG) for i in range(2)]
        tot_2 = [sb("tot%d" % i, [64, 512], F32, stG) for i in range(2)]
        sq_2 = [sb("sq%d" % i, [64, 512], F32, stG) for i in range(2)]
        ssg_2 = [sb("ssg%d" % i, [64, 16], F32, stG) for i in range(2)]
        cst = [sb("cst%d" % i, [128, 2 * D], F32, stG) for i in range(2)]
        cbf = [sb("cbf%d" % i, [128, 2 * D], BF16, stG) for i in range(2)]
        conv_state = [0]
        gla_step = [0]

        NBLK = 144

        def conv_src(blk):
            if blk < 128:
                return peer_uv[blk * 128:(blk + 1) * 128, :], uv_bf[blk * 128:(blk + 1) * 128, :], 2 * D, None, []
            if blk < 136:
                c_ = blk - 128
                return w_q[c_ * 128:(c_ + 1) * 128, :], wq_dram[c_ * 128:(c_ + 1) * 128, :], 2 * D, ln2[:, c_:c_ + 1], ["wq_dram"]
            c_ = blk - 136
            return w_out[c_ * 128:(c_ + 1) * 128, :], wo_dram[c_ * 128:(c_ + 1) * 128, :], D, None, ["wo_dram"]

        def conv_finish(blk):
            s_ = blk % 2
            src, dst, wd, scl, wk = conv_src(blk)
            if scl is None and blk % 2 == 1:
                P.op("dve", I("tensor_copy", out=cbf[s_][:, 0:wd], in_=cst[s_][:, 0:wd]), ["cst%d" % s_], ["cbf%d" % s_])
            elif scl is None:
                P.op("act", I("activation", out=cbf[s_][:, 0:wd], in_=cst[s_][:, 0:wd], func=AF.Copy), ["cst%d" % s_], ["cbf%d" % s_])
            else:
                P.op("act", I("activation", out=cbf[s_][:, 0:wd], in_=cst[s_][:, 0:wd], func=AF.Copy, scale=scl), ["cst%d" % s_, "ln2"], ["cbf%d" % s_])
            P.dma("pool", I("dma_start", out=dst, in_=cbf[s_][:, 0:wd]), ["cbf%d" % s_], wk)

        def conv_blocks(n):
            for _ in range(n):
                blk = conv_state[0]
                if blk > NBLK:
                    return
                conv_state[0] += 1
                if blk < NBLK:
                    s_ = blk % 2
                    src, dst, wd, scl, wk = conv_src(blk)
                    P.dma("pool", I("dma_start", out=cst[s_][:, 0:wd], in_=src), [], ["cst%d" % s_])
                if blk >= 1:
                    conv_finish(blk - 1)

        def gla_chunk(c, dirn, first):
            pp = gla_step[0] % 2
            gla_step[0] += 1
            xc = xc_2[pp]
            xcs = xcs_2[pp]
            xTc = xTc_2[pp]
            gqk = gqk_2[pp]
            gv_bf = gv_bf_2[pp]
            sgr = sgr_2[pp]
            z_sb = z_sb_2[pp]
            zT_sb = zT_sb_2[pp]
            e_sb = e_sb_2[pp]
            l_sb = l_sb_2[pp]
            Lc_sb = Lc_sb_2[pp]
            Dm_sb = Dm_sb_2[pp]
            Eq = Eq_2[pp]
            Ek = Ek_2[pp]
            Eh = Eh_2[pp]
            qt_bf = qt_bf_2[pp]
            kt_bf = kt_bf_2[pp]
            kh_bf = kh_bf_2[pp]
            qkTc = qkTc_2[pp]
            attn_bf = attn_bf_2[pp]
            dec_sb = dec_sb_2[pp]
            tot = tot_2[pp]
            sq = sq_2[pp]
            ssg = ssg_2[pp]
            Pc = 16 if c == 0 else 64
            rows = meta if c == 0 else x[(c - 1) * 64:c * 64, :]
            need_o = (c >= 1)
            P.dma("sp", I("dma_start", out=xc[0:Pc, :], in_=rows), [], [("xc%d" % pp)])
            r = rms(xc[0:Pc, :], Pc, D, "statG%d" % pp, 4 * pp, ("xc%d" % pp))
            P.op("dve", I("tensor_scalar", out=xcs[0:Pc, :], in0=xc[0:Pc, :], scalar1=r, scalar2=None, op0=ALU.mult), [("xc%d" % pp), "statG%d" % pp], [("xcs%d" % pp)])
            pT = psb[0][:].bitcast(BF16)
            for k in range(8):
                P.op("pe", I("transpose", out=pT[:, k * 64:k * 64 + Pc], in_=xcs[0:Pc, k * 128:(k + 1) * 128], identity=identb[0:Pc, 0:Pc]),
                     [("xcs%d" % pp), "identb"], [PSK(0)], inc=(k == 7))
            P.op("act", I("activation", out=xTc[:, :, 0:Pc], in_=pT[:, 0:512].rearrange("p (k n) -> p k n", k=8)[:, :, 0:Pc], func=AF.Copy), [PSK(0)], [("xTc%d" % pp)])
            groups = [(1, 1536, 512), (2, 2048, 512), (4, 3072, 32)]
            if dirn == 1:
                groups.append((3, 2560, 512))
            for bank, c0, ncol in groups:
                for k in range(8):
                    P.op("pe", I("matmul", psb[bank][0:Pc, 0:ncol], lhsT=xTc[:, k, 0:Pc], rhs=w_bf[:, k, c0:c0 + ncol], start=(k == 0), stop=(k == 7)),
                         [("xTc%d" % pp), "w_bf"], [PSK(bank)], inc=(k == 7))
            P.op("act", I("activation", out=gqk[0:Pc, 0:256], in_=psb[1][0:Pc, 0:256], func=AF.Copy, scale=0.125), [PSK(1)], [("gqk%d" % pp)])
            P.op("act", I("activation", out=gqk[0:Pc, 256:512], in_=psb[1][0:Pc, 256:512], func=AF.Copy), [PSK(1)], [("gqk%d" % pp)])
            P.op("act", I("activation", out=gv_bf[0:Pc, :], in_=psb[2][0:Pc, :], func=AF.Copy), [PSK(2)], [("gv_bf%d" % pp)])
            P.op("dve", I("tensor_copy", out=z_sb[0:Pc, :], in_=psb[4][0:Pc, 0:32]), [PSK(4)], [("z_sb%d" % pp)])
            if dirn == 1:
                P.op("act", I("activation", out=sq[0:Pc, :], in_=psb[3][0:Pc, :], func=AF.Exp, scale=-1.0), [PSK(3)], [("sq%d" % pp)])
                P.op("act", I("activation", out=sq[0:Pc, :], in_=sq[0:Pc, :], func=AF.Ln, bias=1.0), [("sq%d" % pp)], [("sq%d" % pp)])
                P.op("act", I("activation", out=sq[0:Pc, :], in_=sq[0:Pc, :], func=AF.Exp, scale=-1.0), [("sq%d" % pp)], [("sq%d" % pp)])
                P.op("dve", I("tensor_tensor", out=sgr[0:Pc, :], in0=psb[3][0:Pc, :], in1=sq[0:Pc, :], op=ALU.mult), [PSK(3), ("sq%d" % pp)], [("sgr%d" % pp)])
            P.op("pe", I("transpose", out=psb[4][0:32, 32:32 + Pc], in_=z_sb[0:Pc, 0:32], identity=identf[0:Pc, 0:Pc]), [("z_sb%d" % pp), "identf"], [PSK(4)])
            P.op("dve", I("tensor_copy", out=zT_sb[:, 0:Pc], in_=psb[4][0:32, 32:32 + Pc]), [PSK(4)], [("zT_sb%d" % pp)])
            g0 = dirn * 256
            P.op("pe", I("matmul", psb[5][0:Pc, 0:256], lhsT=zT_sb[:, 0:Pc], rhs=w2c[:, g0:g0 + 256], start=True, stop=False), [("zT_sb%d" % pp), "w2c"], [PSK(5)], inc=False)
            P.op("pe", I("matmul", psb[5][0:Pc, 0:256], lhsT=ones_row[0:1, 0:Pc], rhs=bc[0:1, g0:g0 + 256], start=False, stop=True), ["ones_row", "bc"], [PSK(5)])
            P.op("act", I("activation", out=e_sb[0:Pc, :], in_=psb[5][0:Pc, 0:256], func=AF.Exp, scale=-1.0), [PSK(5)], [("e_sb%d" % pp)])
            P.op("act", I("activation", out=l_sb[0:Pc, :], in_=e_sb[0:Pc, :], func=AF.Ln, bias=1.0), [("e_sb%d" % pp)], [("l_sb%d" % pp)])
            triD = triF if dirn == 0 else triB
            P.op("pe", I("matmul", psb[6][0:Pc, 0:256], lhsT=triD[0:Pc, 0:Pc], rhs=l_sb[0:Pc, :], start=True, stop=True), ["tri", ("l_sb%d" % pp)], [PSK(6)], inc=False)
            P.op("pe", I("matmul", psb[6][0:Pc, 256:512], lhsT=ones64[0:Pc, 0:Pc], rhs=l_sb[0:Pc, :], start=True, stop=True), ["tri", ("l_sb%d" % pp)], [PSK(6)])
            for hh in range(4):
                P.op("pe", I("matmul", psb[4][0:64, 96 + hh:97 + hh], lhsT=l_sb[0:Pc, hh * 64:(hh + 1) * 64], rhs=onescol[0:Pc, 0:1], start=True, stop=True),
                     [("l_sb%d" % pp), "tri"], [PSK(4)], inc=(hh == 3))
            P.op("act", I("activation", out=dec_sb[:, :], in_=psb[4][0:64, 96:100], func=AF.Exp, scale=-1.0 / 16), [PSK(4)], [("dec_sb%d" % pp)])
            if need_o:
                P.op("act", I("activation", out=Eq[0:Pc, :], in_=psb[6][0:Pc, 0:256], func=AF.Exp, scale=-1.0 / 16), [PSK(6)], [("Eq%d" % pp)])
                P.op("act", I("activation", out=Ek[0:Pc, :], in_=psb[6][0:Pc, 0:256], func=AF.Exp, scale=1.0 / 16), [PSK(6)], [("Ek%d" % pp)])
            P.op("act", I("activation", out=Lc_sb[0:Pc, :], in_=psb[6][0:Pc, 0:256], func=AF.Copy), [PSK(6)], [("Lc_sb%d" % pp)])
            P.op("dve", I("tensor_tensor", out=Dm_sb[0:Pc, :], in0=psb[6][0:Pc, 256:512], in1=Lc_sb[0:Pc, :], op=ALU.subtract), [PSK(6), ("Lc_sb%d" % pp)], [("Dm_sb%d" % pp)])
            P.op("act", I("activation", out=Eh[0:Pc, :], in_=Dm_sb[0:Pc, :], func=AF.Exp, scale=-1.0 / 16), [("Dm_sb%d" % pp)], [("Eh%d" % pp)])
            P.op("dve", I("tensor_tensor", out=kh_bf[0:Pc, :], in0=gqk[0:Pc, 256:512], in1=Eh[0:Pc, :], op=ALU.mult), [("gqk%d" % pp), ("Eh%d" % pp)], [("kh_bf%d" % pp)])
            if need_o:
                P.op("dve", I("tensor_tensor", out=qt_bf[0:Pc, :], in0=gqk[0:Pc, 0:256], in1=Eq[0:Pc, :], op=ALU.mult), [("gqk%d" % pp), ("Eq%d" % pp)], [("qt_bf%d" % pp)])
                P.op("dve", I("tensor_tensor", out=kt_bf[0:Pc, :], in0=gqk[0:Pc, 256:512], in1=Ek[0:Pc, :], op=ALU.mult), [("gqk%d" % pp), ("Ek%d" % pp)], [("kt_bf%d" % pp)])
                p7 = psb[7][:].bitcast(BF16)
                for hh in range(4):
                    P.op("pe", I("transpose", out=p7[0:64, hh * 64:hh * 64 + Pc], in_=qt_bf[0:Pc, hh * 64:(hh + 1) * 64], identity=identb[0:Pc, 0:Pc]),
                         [("qt_bf%d" % pp), "identb"], [PSK(7)], inc=False)
                for hh in range(4):
                    P.op("pe", I("transpose", out=p7[0:64, (4 + hh) * 64:(4 + hh) * 64 + Pc], in_=kt_bf[0:Pc, hh * 64:(hh + 1) * 64], identity=identb[0:Pc, 0:Pc]),
                         [("kt_bf%d" % pp), "identb"], [PSK(7)], inc=(hh == 3))
                P.op("act", I("activation", out=qkTc[:, :, 0:Pc], in_=p7[0:64, 0:512].rearrange("p (k n) -> p k n", k=8)[:, :, 0:Pc], func=AF.Copy), [PSK(7)], [("qkTc%d" % pp)])
                psA = psb[7][0:64, 256:512].rearrange("p (h n) -> p h n", h=4)
                for hh in range(4):
                    P.op("pe", I("matmul", psA[0:Pc, hh, 0:Pc], lhsT=qkTc[:, 4 + hh, 0:Pc], rhs=qkTc[:, hh, 0:Pc], start=True, stop=True),
                         [("qkTc%d" % pp)], [PSK(7)], inc=(hh == 3))
                P.op("dve", I("tensor_tensor", out=attn_bf[0:Pc, :, 0:Pc], in0=psA[0:Pc, :, 0:Pc], in1=triD[0:Pc, 0:Pc].unsqueeze(1).broadcast_to([Pc, 4, Pc]), op=ALU.mult),
                     [PSK(7), "tri"], [("attn_bf%d" % pp)])
                psO = psb[1][0:64, :].rearrange("p (h n) -> p h n", h=4)
                for hh in range(4):
                    P.op("pe", I("matmul", psO[0:Pc, hh, :], lhsT=attn_bf[0:Pc, hh, 0:Pc], rhs=gv_bf[0:Pc, hh * 128:(hh + 1) * 128], start=True, stop=first),
                         [("attn_bf%d" % pp), ("gv_bf%d" % pp)], [PSK(1)], inc=(first and hh == 3))
                    if not first:
                        P.op("pe", I("matmul", psO[0:Pc, hh, :], lhsT=qkTc[:, hh, 0:Pc], rhs=S_bf[:, hh, :], start=False, stop=True),
                             [("qkTc%d" % pp), "S_bf"], [PSK(1)], inc=(hh == 3))
            psP = psb[2][0:64, :].rearrange("p (h n) -> p h n", h=4)
            for hh in range(4):
                P.op("pe", I("matmul", psP[:, hh, :], lhsT=kh_bf[0:Pc, hh * 64:(hh + 1) * 64], rhs=gv_bf[0:Pc, hh * 128:(hh + 1) * 128], start=True, stop=True),
                     [("kh_bf%d" % pp), ("gv_bf%d" % pp)], [PSK(2)], inc=(hh == 3))
            if need_o:
                if dirn == 0:
                    P.op("act", I("activation", out=og[0:64, c - 1, :], in_=psb[1][0:64, :], func=AF.Copy), [PSK(1)], ["og"])
                else:
                    P.op("dve", I("tensor_tensor", out=tot[:, :], in0=psb[1][0:64, :], in1=og[0:64, c - 1, :], op=ALU.add), [PSK(1), "og"], [("tot%d" % pp)])
                    P.op("dve", I("tensor_tensor", out=sq[:, :], in0=tot[:, :], in1=tot[:, :], op=ALU.mult), [("tot%d" % pp)], [("sq%d" % pp)])
                    P.op("dve", I("tensor_reduce", out=ssg[:, 0:4], in_=sq[:, :].rearrange("p (h n) -> p h n", h=4), axis=AX.X, op=ALU.add), [("sq%d" % pp)], [("ssg%d" % pp)])
                    P.op("act", I("activation", out=ssg[:, 4:8], in_=ssg[:, 0:4], func=AF.Ln, bias=EPS, scale=1.0 / 128), [("ssg%d" % pp)], [("ssg%d" % pp)])
                    P.op("act", I("activation", out=ssg[:, 8:12], in_=ssg[:, 4:8], func=AF.Exp, scale=-0.5), [("ssg%d" % pp)], [("ssg%d" % pp)])
                    t3 = tot[:, :].rearrange("p (h n) -> p h n", h=4)
                    P.op("dve", I("tensor_tensor", out=t3, in0=t3, in1=ssg[:, 8:12].unsqueeze(2).broadcast_to([64, 4, 128]), op=ALU.mult), [("tot%d" % pp), ("ssg%d" % pp)], [("tot%d" % pp)])
                    P.op("dve", I("tensor_tensor", out=t3, in0=t3, in1=glanb[0:64, :].unsqueeze(1).broadcast_to([64, 4, 128]), op=ALU.mult), [("tot%d" % pp), "glanb"], [("tot%d" % pp)])
                    P.op("dve", I("tensor_tensor", out=og[0:64, c - 1, :], in0=tot[:, :], in1=sgr[:, :], op=ALU.mult), [("tot%d" % pp), ("sgr%d" % pp)], ["og"])
            if first:
                P.op("dve", I("tensor_copy", out=S_f[:, :, :], in_=psP), [PSK(2)], ["S_f"])
            else:
                P.op("dve", I("tensor_tensor", out=S_f[:, :, :], in0=S_f[:, :, :], in1=dec_sb[:, :].unsqueeze(2).broadcast_to([64, 4, 128]), op=ALU.mult),
                     ["S_f", ("dec_sb%d" % pp)], ["S_f"])
                P.op("dve", I("tensor_tensor", out=S_f[:, :, :], in0=S_f[:, :, :], in1=psP, op=ALU.add), ["S_f", PSK(2)], ["S_f"])
            P.op("act", I("activation", out=S_bf[:, :, :], in_=S_f[:, :, :], func=AF.Copy), ["S_f"], ["S_bf"])

        sched = [(c, 0, c == 0) for c in range(0, 33)] + [(c, 1, c == 32) for c in range(32, 0, -1)]
        lists = []
        for (c, d_, fi) in sched:
            P.rec = []
            gla_chunk(c, d_, fi)
            lists.append(P.rec)
            P.rec = None

        def touches_state(r):
            return any(k in ("S_bf", "S_f") for k in r[3]) or any(k in ("S_bf", "S_f") for k in r[4])

        cur = lists[0]
        ci = 0
        for r in cur[:len(cur) // 2]:
            P.replay(r)
        ci = len(cur) // 2
        for n_ in range(len(lists)):
            cur = lists[n_]
            nxt = lists[n_ + 1] if n_ + 1 < len(lists) else []
            half_n = len(nxt) // 2
            ni = 0
            while ci < len(cur) or ni < half_n:
                if ci < len(cur):
                    P.replay(cur[ci])
                    ci += 1
                    while ci < len(cur) and cur[ci - 1][1] == "pe" and cur[ci - 1][5] is False:
                        P.replay(cur[ci])
                        ci += 1
                if ni < half_n:
                    if touches_state(nxt[ni]) and ci < len(cur):
                        continue
                    P.replay(nxt[ni])
                    ni += 1
                    while ni < half_n and nxt[ni - 1][1] == "pe" and nxt[ni - 1][5] is False:
                        P.replay(nxt[ni])
                        ni += 1
            ci = ni
            conv_blocks(3 if n_ < 16 else 2)
        conv_blocks(NBLK + 2)
        if dbg is not None and dbg[0] == "C":
            dt_ = sb("dbgt", [64, 32 * 512], F32)
            P.op("dve", I("tensor_copy", out=dt_[:], in_=og[:].rearrange("p t c -> p (t c)")), ["og"], ["dbgt"])
            P.dma("sp", I("dma_start", out=dbg_out, in_=dt_[:]), ["dbgt"], [], final=True)
            P.emit()
            return nc
        stG.close()
        st1.close()
        stK.close()
        P.barrier()

        keys_sb = sb("keys_sb", [128, 2048], F32)
        ln2b = sb("ln2b", [128, D], F32)
        finb = sb("finb", [128, D], F32)
        ld("act", keys_sb[:], keysT, "keys_sb")
        ld("act", ln2b[:], ln2_b, "ln2b")
        ld("act", finb[:], fin_b, "finb")
        wpiece = [0]
        wout_bf = sb("wout_bf", [128, 8, 1024], BF16)
        for nb in range(2):
            P.dma("sp", I("dma_start", out=wout_bf[:, :, nb * 512:(nb + 1) * 512], in_=wo_dram[:, nb * 512:(nb + 1) * 512].rearrange("(k p) c -> p k c", p=128)),
                  ["wo_dram"], ["wout_bf"])
        wqs = [sb("wqs%d" % i, [128, 8, 512], BF16) for i in range(2)]
        mixT = sb("mixT", [128, 8, 128], BF16)
        h2_2 = [sb("h2_%d" % i, [128, D], F32) for i in range(2)]
        hn_2 = [sb("hn_%d" % i, [128, D], BF16) for i in range(2)]
        hs_bf = sb("hs_bf", [128, D], BF16)
        hT = sb("hT", [128, 8, 128], BF16)
        qT_sb = sb("qT_sb", [128, 16, 128], F32)
        s_sb = sb("s_sb", [128, 16, 128], F32)
        ts = sb("ts", [128, 16, 16], F32)
        ti = sb("ti", [128, 16, 16], U32)
        tif = sb("tif", [128, 16, 16], F32)
        cand = sb("cand", [128, 2, 256], F32)
        bs = sb("bs", [128, 8, 16], F32)
        pos = sb("pos", [128, 8, 16], U32)
        idxf = sb("idxf", [128, 128], F32)
        idxi_2 = [sb("idxi_%d" % i, [128, 128], I32) for i in range(2)]
        gate_2 = [sb("gate_%d" % i, [128, 128], F32) for i in range(2)]
        prodb = [sb("prodb%d" % i, [128, D], BF16) for i in range(3)]
        gz = sb("gz", [128, 16], F32)
        apre = sb("apre", [128, 128], F32)
        gel = sb("gel", [128, 128], F32)
        s_bfv = s_sb[:, :, :].rearrange("p a b -> p (a b)").bitcast(BF16)
        prod = [s_bfv[:, i * 1024:(i + 1) * 1024] for i in range(4)]
        ai = sb("ai", [128, 128], U32)
        af = sb("af", [128, 2, 128], F32)
        rsel = sb("rsel", [128, 2, 128], F32)
        iota16 = iota[:, 0:16]
        diag = [sb("diag%d" % i, [128, 128], BF16) for i in range(4)]
        NG = 13
        gbuf = [sb("gbuf%d" % i, [128, 2 * D], BF16) for i in range(NG)]
        og_flat = og_full[:, :, :].rearrange("p c n -> p (c n)")
        gextra = [og_flat[:, j * 2048:(j + 1) * 2048] for j in range(6)]
        gextra_first = [True] * 6
        outsb = qT_sb[:, 0:8, :].rearrange("p a b -> p (a b)")
        gi = 0

        INTERLEAVE = 2
        L1 = []
        L1a = []
        L2 = []
        for t in range(16):
            tp = t % 2
            h2 = h2_2[tp]
            hn = hn_2[tp]
            idxi = idxi_2[tp]
            gate = gate_2[tp]
            P.rec = []
            pT = psb[0][:].bitcast(BF16)
            for k in range(4):
                P.op("pe", I("transpose", out=pT[:, k * 128:(k + 1) * 128], in_=mix_da[:, t, k * 128:(k + 1) * 128], identity=identb[:, :]),
                     ["mix_da", "identb"], [PSK(0)], inc=False)
            for half in range(2):
                for k in range(4):
                    P.op("pe", I("transpose", out=pT[:, (4 + k) * 128 + half * 64:(4 + k) * 128 + half * 64 + 64], in_=og[0:64, 2 * t + half, k * 128:(k + 1) * 128],
                                 identity=identb[0:64, 0:64]), ["og", "identb"], [PSK(0)], inc=(half == 1 and k == 3))
            P.op("act", I("activation", out=mixT[:, :, :], in_=pT.rearrange("p (k n) -> p k n", k=8), func=AF.Copy), [PSK(0)], ["mixT"])
            P.dma("sp", I("dma_start", out=h2[:, :], in_=x[t * 128:(t + 1) * 128, :]), [], [("h2_%d" % tp)])
            for nb in range(2):
                for k in range(8):
                    P.op("pe", I("matmul", psb[1 + nb][:, :], lhsT=mixT[:, k, :], rhs=wout_bf[:, k, nb * 512:(nb + 1) * 512], start=(k == 0), stop=(k == 7)),
                         ["mixT", "wout_bf"], [PSK(1 + nb)], inc=(k == 7))
                P.op("dve", I("tensor_tensor", out=h2[:, nb * 512:(nb + 1) * 512], in0=h2[:, nb * 512:(nb + 1) * 512], in1=psb[1 + nb][:, :], op=ALU.add),
                     [("h2_%d" % tp), PSK(1 + nb)], [("h2_%d" % tp)])
            r = rms(h2[:, :], 128, D, "statE", 0, ("h2_%d" % tp))
            P.op("dve", I("scalar_tensor_tensor", out=hn[:, :], in0=h2[:, :], scalar=r, in1=ln2b[:, :], op0=ALU.mult, op1=ALU.mult), [("h2_%d" % tp), "statE", "ln2b"], [("hn_%d" % tp)])
            P.op("act", I("activation", out=hs_bf[:, :], in_=h2[:, :], func=AF.Copy, scale=r), [("h2_%d" % tp), "statE"], ["hs_bf"])
            pT3 = psb[3][:].bitcast(BF16)
            for k in range(8):
                P.op("pe", I("transpose", out=pT3[:, k * 128:(k + 1) * 128], in_=hs_bf[:, k * 128:(k + 1) * 128], identity=identb[:, :]),
                     ["hs_bf", "identb"], [PSK(3)], inc=(k == 7))
            P.op("act", I("activation", out=hT[:, :, :], in_=pT3.rearrange("p (k n) -> p k n", k=8), func=AF.Copy), [PSK(3)], ["hT"])
            for g4 in range(4):
                bank = 4 + (g4 % 2)
                ws = wpiece[0] % 2
                wpiece[0] += 1
                P.dma("sp", I("dma_start", out=wqs[ws][:, :, :], in_=wq_dram[:, g4 * 512:(g4 + 1) * 512].rearrange("(k p) c -> p k c", p=128)), ["wq_dram"], ["wqs%d" % ws])
                for j in range(4):
                    hp = g4 * 4 + j
                    for k in range(8):
                        P.op("pe", I("matmul", psb[bank][:, j * 128:(j + 1) * 128], lhsT=wqs[ws][:, k, j * 128:(j + 1) * 128], rhs=hT[:, k, :], start=(k == 0), stop=(k == 7)),
                             ["wqs%d" % ws, "hT"], [PSK(bank)], inc=(k == 7))
                if g4 % 2 == 0:
                    P.op("act", I("activation", out=qT_sb[:, g4 * 4:(g4 + 1) * 4, :], in_=psb[bank][:, :].rearrange("p (j n) -> p j n", j=4), func=AF.Copy), [PSK(bank)], ["qT_sb"])
                else:
                    P.op("dve", I("tensor_copy", out=qT_sb[:, g4 * 4:(g4 + 1) * 4, :], in_=psb[bank][:, :].rearrange("p (j n) -> p j n", j=4)), [PSK(bank)], ["qT_sb"])
            for g4 in range(4):
                bank = g4
                for j in range(4):
                    hp = g4 * 4 + j
                    P.op("pe", I("matmul", psb[bank][:, j * 128:(j + 1) * 128], lhsT=qT_sb[:, hp, :], rhs=keys_sb[:, hp * 128:(hp + 1) * 128], start=True, stop=True),
                         ["qT_sb", "keys_sb"], [PSK(bank)], inc=(j == 3))
                if g4 % 2 == 0:
                    P.op("act", I("activation", out=s_sb[:, g4 * 4:(g4 + 1) * 4, :], in_=psb[bank][:, :].rearrange("p (j n) -> p j n", j=4), func=AF.Copy), [PSK(bank)], ["s_sb"])
                else:
                    P.op("dve", I("tensor_copy", out=s_sb[:, g4 * 4:(g4 + 1) * 4, :], in_=psb[bank][:, :].rearrange("p (j n) -> p j n", j=4)), [PSK(bank)], ["s_sb"])
            L1a.append(P.rec)
            P.rec = []
            for hp in range(16):
                P.op("dve", I("max", out=ts[:, hp, 0:8], in_=s_sb[:, hp, :]), ["s_sb"], ["ts"])
                P.op("dve", I("max_index", out=ti[:, hp, 0:8], in_max=ts[:, hp, 0:8], in_values=s_sb[:, hp, :]), ["s_sb", "ts"], ["ti"])
                P.op("dve", I("match_replace", out=s_sb[:, hp, :], in_to_replace=ts[:, hp, 0:8], in_values=s_sb[:, hp, :], imm_value=NEG), ["s_sb", "ts"], ["s_sb"])
                P.op("dve", I("max", out=ts[:, hp, 8:16], in_=s_sb[:, hp, :]), ["s_sb"], ["ts"])
                P.op("dve", I("max_index", out=ti[:, hp, 8:16], in_max=ts[:, hp, 8:16], in_values=s_sb[:, hp, :]), ["s_sb", "ts"], ["ti"])
            P.op("dve", I("tensor_copy", out=tif[:, :, :], in_=ti[:, :, :]), ["ti"], ["tif"])
            for h in range(8):
                cb = h % 2
                ck = "cand%d" % cb
                c3 = cand[:, cb, :].rearrange("p (a b) -> p a b", a=16)
                P.op("dve", I("tensor_tensor", out=c3, in0=ts[:, 2 * h, :].unsqueeze(2).broadcast_to([128, 16, 16]),
                              in1=ts[:, 2 * h + 1, :].unsqueeze(1).broadcast_to([128, 16, 16]), op=ALU.add), ["ts"], [ck])
                P.op("dve", I("max", out=bs[:, h, 0:8], in_=cand[:, cb, :]), [ck], ["bs"])
                P.op("dve", I("max_index", out=pos[:, h, 0:8], in_max=bs[:, h, 0:8], in_values=cand[:, cb, :]), [ck, "bs"], ["pos"])
                P.op("dve", I("match_replace", out=cand[:, cb, :], in_to_replace=bs[:, h, 0:8], in_values=cand[:, cb, :], imm_value=NEG), [ck, "bs"], [ck])
                P.op("dve", I("max", out=bs[:, h, 8:16], in_=cand[:, cb, :]), [ck], ["bs"])
                P.op("dve", I("max_index", out=pos[:, h, 8:16], in_max=bs[:, h, 8:16], in_values=cand[:, cb, :]), [ck, "bs"], ["pos"])
            pos2 = pos[:, :, :].rearrange("p h k -> p (h k)")
            P.op("dve", I("tensor_single_scalar", out=ai[:, :], in_=pos2, scalar=4, op=ALU.logical_shift_right), ["pos"], ["ai"])
            P.op("dve", I("tensor_copy", out=af[:, 0, :], in_=ai[:, :]), ["ai"], ["af"])
            P.op("dve", I("tensor_single_scalar", out=ai[:, :], in_=pos2, scalar=15, op=ALU.bitwise_and), ["pos", "af"], ["ai"])
            P.op("dve", I("tensor_copy", out=af[:, 1, :], in_=ai[:, :]), ["ai"], ["af"])
            for li in range(2):
                for half in range(2):
                    oh = prod[half].rearrange("p (a b) -> p a b", b=16)
                    oh4 = prod[half].rearrange("p (h k b) -> p h k b", h=4, k=16)
                    okey = "s_sb"
                    P.op("dve", I("tensor_tensor", out=oh, in0=af[:, li, half * 64:(half + 1) * 64].unsqueeze(2).broadcast_to([128, 64, 16]),
                                  in1=iota16.unsqueeze(1).broadcast_to([128, 64, 16]), op=ALU.is_equal), ["af", "iota"], [okey])
                    tl = tif[:, half * 8:(half + 1) * 8, :].rearrange("p (h two) a -> p h two a", two=2)[:, :, li, :]
                    P.op("dve", I("tensor_tensor", out=oh4, in0=oh4, in1=tl.unsqueeze(2).broadcast_to([128, 4, 16, 16]), op=ALU.mult), [okey, "tif"], [okey])
                    P.op("dve", I("tensor_reduce", out=rsel[:, li, half * 64:(half + 1) * 64], in_=oh, axis=AX.X, op=ALU.add), [okey], ["rsel"])
            P.op("dve", I("scalar_tensor_tensor", out=idxf[:, :], in0=rsel[:, 0, :], scalar=128.0, in1=rsel[:, 1, :], op0=ALU.mult, op1=ALU.add), ["rsel"], ["idxf"])
            P.op("dve", I("tensor_copy", out=idxi[:, :], in_=idxf[:, :]), ["idxf"], [("idxi_%d" % tp)])
            g3 = gate[:, :].rearrange("p (h k) -> p h k", h=8)
            P.op("dve", I("tensor_tensor", out=g3, in0=bs[:, :, :], in1=bs[:, :, 0:1].broadcast_to([128, 8, 16]), op=ALU.subtract), ["bs"], [("gate_%d" % tp)])
            P.op("act", I("activation", out=gate[:, :], in_=gate[:, :], func=AF.Exp), [("gate_%d" % tp)], [("gate_%d" % tp)])
            P.op("dve", I("tensor_reduce", out=gz[:, 0:8], in_=g3, axis=AX.X, op=ALU.add), [("gate_%d" % tp)], ["gz"])
            P.op("dve", I("reciprocal", out=gz[:, 8:16], in_=gz[:, 0:8]), ["gz"], ["gz"])
            P.op("dve", I("tensor_tensor", out=g3, in0=g3, in1=gz[:, 8:16].unsqueeze(2).broadcast_to([128, 8, 16]), op=ALU.mult), [("gate_%d" % tp), "gz"], [("gate_%d" % tp)])
            L1.append(P.rec)
            P.rec = []
            LAG = 4
            gi0 = gi
            n_extra = max(0, min(6, (t - 2) // 2))
            bl = [(gbuf[i][:, :], "gbuf%d" % i, None) for i in range(NG)] + [(gextra[j], "gx%d" % j, j) for j in range(n_extra)]
            for step in range(128 + LAG):
                if step < 128:
                    hk = step
                    gb, gk, gj = bl[hk % len(bl)]
                    gw = [gk]
                    if gj is not None and gextra_first[gj]:
                        gextra_first[gj] = False
                        gw = [gk, "og"]
                    P.dma("pool", I("indirect_dma_start", out=gb, out_offset=None, in_=uv_bf[:, :],
                                    in_offset=bass.IndirectOffsetOnAxis(ap=idxi[:, hk:hk + 1], axis=0)), [("idxi_%d" % tp)], gw)
                    pb = hk % 3
                    P.op("dve", I("tensor_tensor", out=prodb[pb][:, :], in0=gb[:, 0:D], in1=hn[:, :], op=ALU.mult), [gk, ("hn_%d" % tp)], ["prodb%d" % pb])
                    P.op("act", I("activation", out=junkb[:, :], in_=prodb[pb][:, :], func=AF.Copy, accum_out=apre[:, hk:hk + 1]), ["prodb%d" % pb], ["junkb", "apre%d" % hk])
                    P.op("act", I("activation", out=gel[:, hk:hk + 1], in_=apre[:, hk:hk + 1], func=AF.Gelu), ["apre%d" % hk], ["gel%d" % hk])
                if step >= LAG:
                    hk = step - LAG
                    gb, gk, gj = bl[hk % len(bl)]
                    dg = hk % 4
                    P.op("dve", I("tensor_scalar", out=diag[dg][:, :], in0=identf[:, :], scalar1=gel[:, hk:hk + 1], scalar2=gate[:, hk:hk + 1], op0=ALU.mult, op1=ALU.mult),
                         ["identf", "gel%d" % hk, ("gate_%d" % tp)], ["diag%d" % dg])
                    for nb in range(2):
                        P.op("pe", I("matmul", psb[6 + nb][:, :], lhsT=diag[dg][:, :], rhs=gb[:, D + nb * 512:D + (nb + 1) * 512], start=(hk == 0), stop=(hk == 127)),
                             ["diag%d" % dg, gk], [PSK(6 + nb)], inc=(nb == 1))
            gi = gi0 + 128
            for nb in range(2):
                P.op("dve", I("tensor_tensor", out=h2[:, nb * 512:(nb + 1) * 512], in0=h2[:, nb * 512:(nb + 1) * 512], in1=psb[6 + nb][:, :], op=ALU.add),
                     [("h2_%d" % tp), PSK(6 + nb)], [("h2_%d" % tp)])
            r = rms(h2[:, :], 128, D, "statF", 4, ("h2_%d" % tp))
            P.op("dve", I("scalar_tensor_tensor", out=outsb, in0=h2[:, :], scalar=r, in1=finb[:, :], op0=ALU.mult, op1=ALU.mult), [("h2_%d" % tp), "statF", "finb"], ["qT_sb"])
            P.dma("sp", I("dma_start", out=out[t * 128:(t + 1) * 128, :], in_=outsb), ["qT_sb"], [], final=True)
            L2.append(P.rec)
            P.rec = None

        def replay_group(lst, i):
            P.replay(lst[i])
            i += 1
            while i < len(lst) and lst[i - 1][0] == "op" and lst[i - 1][1] == "pe" and lst[i - 1][5] is False:
                P.replay(lst[i])
                i += 1
            return i

        def replay_all(lst):
            i = 0
            while i < len(lst):
                i = replay_group(lst, i)

        replay_all(L1a[0])
        replay_all(L1[0])
        for t in range(16):
            if t + 1 < 16:
                replay_all(L1a[t + 1])
            cur = L2[t]
            nxt = L1[t + 1] if t + 1 < 16 else []
            ci = 0
            ni = 0
            cnt = 0
            while ci < len(cur):
                ci = replay_group(cur, ci)
                cnt += 1
                if INTERLEAVE and cnt % INTERLEAVE == 0 and ni < len(nxt):
                    ni = replay_group(nxt, ni)
            while ni < len(nxt):
                ni = replay_group(nxt, ni)
        P.emit()
    return nc


def _consts():
    c = {}
    c["c_identb"] = np.eye(128, dtype=np.float32).astype(ml_dtypes.bfloat16)
    c["c_identf"] = np.eye(128, dtype=np.float32)
    tri = np.zeros((64, 256), np.float32)
    j = np.arange(64)[:, None]
    i = np.arange(64)[None, :]
    tri[:, 0:64] = (j <= i)
    tri[:, 64:128] = (j >= i)
    tri[:, 128:192] = 1.0
    tri[:, 192] = 1.0
    c["c_tri"] = tri
    inv = np.power(np.float32(500000.0), -np.arange(0, 16, 2, dtype=np.float32) / np.float32(16)).astype(np.float32)
    cs = np.zeros((128, 17 * 16), np.float32)
    for t in range(17):
        if t < 16:
            pos = 16 + t * 128 + np.arange(128)
        else:
            pos = np.concatenate([np.arange(16), np.zeros(112)])
        ang = pos.astype(np.float32)[:, None] * inv[None, :]
        cs[:, t * 16:t * 16 + 8] = np.cos(ang)
        cs[:, t * 16 + 8:t * 16 + 16] = np.sin(ang)
    c["c_cs"] = cs
    c["c_iota"] = np.broadcast_to(np.arange(256, dtype=np.float32)[None, :], (128, 256)).copy()
    return c


def _prep(inp):
    f = lambda a: np.ascontiguousarray(np.asarray(a, dtype=np.float32))
    bro = lambda v, n: np.ascontiguousarray(np.broadcast_to(f(v).reshape(1, -1), (128, n)))
    m = {}
    m["meta"] = f(inp["meta_tokens"])
    m["w_in"] = f(inp["w_in"][0])
    m["ln1c"] = f(np.asarray(inp["ln1_w"][0]).reshape(8, 128).T)
    m["w_out"] = f(inp["w_out"][0])
    m["ln2c"] = f(np.asarray(inp["ln2_w"][0]).reshape(8, 128).T)
    m["ln2_b"] = bro(inp["ln2_w"][0], 1024)
    m["fin_b"] = bro(inp["final_norm_w"], 1024)
    m["w_q"] = f(inp["peer_w_q"][0])
    sk = np.asarray(inp["peer_sub_keys"][0], dtype=np.float32)
    m["keysT"] = f(sk.transpose(3, 0, 1, 2).reshape(128, 2048))
    m["peer_uv"] = np.ascontiguousarray(np.concatenate([np.asarray(inp["peer_u"][0], dtype=np.float32), np.asarray(inp["peer_v"][0], dtype=np.float32)], axis=1))
    m["subln_b"] = bro(inp["da_subln_w"][0], 128)
    m["glan_b"] = bro(inp["gla_norm_w"][0], 128)
    lam = np.concatenate([np.asarray(inp[k][0], dtype=np.float32) for k in ("da_lambda_q1", "da_lambda_k1", "da_lambda_q2", "da_lambda_k2")])
    m["lamv"] = bro(lam, 256)
    w2 = np.zeros((32, 512), np.float32)
    w2[0:16, 0:256] = np.asarray(inp["gla_gate_w2_f"][0])
    w2[16:32, 256:512] = np.asarray(inp["gla_gate_w2_b"][0])
    m["w2cat"] = w2
    m["bcat"] = f(np.concatenate([np.asarray(inp["gla_gate_b_f"][0]), np.asarray(inp["gla_gate_b_b"][0])]).reshape(1, 512))
    m.update(_consts())
    return m


_NC_CACHE = {}


def kernel(**inputs):
    shared = _prep(inputs)
    xs_ = np.asarray(inputs["x"], dtype=np.float32)
    if "nc" not in _NC_CACHE:
        _NC_CACHE["nc"] = build()
    nc = _NC_CACHE["nc"]
    in_maps = []
    for b in range(8):
        m = dict(shared)
        m["x"] = np.ascontiguousarray(xs_[b])
        in_maps.append(m)
    res = run_bass_kernel_spmd(nc, in_maps, core_ids=list(range(8)))
    return np.stack([np.asarray(r["out"], dtype=np.float32) for r in res.results], axis=0)
```
